# Optimizing a Trainium2 kernel written in Bass

```python
import math
import jax, jax.numpy as jnp
from jax import lax
import numpy as np

D_MODEL = 1024
BATCH = 8
SEQ = 2048
DEPTH = 2

GRID_W = 64
CTX_LEN = 256
SSD_WIDTH = D_MODEL // 2
SSD_HEAD_DIM = 64
SSD_HEADS = SSD_WIDTH // SSD_HEAD_DIM
SSD_GROUPS = 2
SSD_STATE = 128
SSD_CONV = 5
SSD_CHUNK = 128
SSD_XBC = SSD_WIDTH + 2 * SSD_GROUPS * SSD_STATE
SSD_COLS = SSD_WIDTH + SSD_XBC + 2 * SSD_HEADS
CONF_WIDTH = D_MODEL // 4
CONF_KERNEL = 31
CONF_COLS = 2 * CONF_WIDTH
S5_WIDTH = D_MODEL // 4
S5_GROUP_CH = 16
S5_GROUPS = S5_WIDTH // S5_GROUP_CH
S5_STATE = 64
S5_COLS = S5_WIDTH
IN_COLS = SSD_COLS + CONF_COLS + S5_COLS
MIX_WIDTH = SSD_WIDTH + CONF_WIDTH + S5_WIDTH
N_EXPERTS = 16
N_EXPERT_GROUPS = 4
EXPERTS_PER_GROUP = N_EXPERTS // N_EXPERT_GROUPS
TOP_K = 2
EXPERT_FF = D_MODEL
N_MOD = 6
EPS = 1e-6
F32 = jnp.float32

kernel_name = "hybrid_ssd_conformer_s5_moe_block"


def rms_norm(x, g):
    x32 = x.astype(F32)
    y = x32 * lax.rsqrt(jnp.mean(x32 * x32, axis=-1, keepdims=True) + EPS)
    return (y * g.astype(F32)).astype(x.dtype)


def layer_norm(x, g, b):
    x32 = x.astype(F32)
    xc = x32 - jnp.mean(x32, axis=-1, keepdims=True)
    y = xc * lax.rsqrt(jnp.mean(xc * xc, axis=-1, keepdims=True) + EPS)
    return (y * g.astype(F32) + b.astype(F32)).astype(x.dtype)


def depthwise_conv(x, w, b):
    k = w.shape[0]
    y = lax.conv_general_dilated(
        x, w[:, None, :].astype(x.dtype), (1,), [(k // 2, k // 2)],
        dimension_numbers=("NWC", "WIO", "NWC"), feature_group_count=x.shape[-1])
    return y + b.astype(x.dtype)


def grid_pos_embed(rows, dtype):
    rr, cc = jnp.meshgrid(jnp.arange(rows, dtype=F32), jnp.arange(GRID_W, dtype=F32), indexing="ij")
    quarter = D_MODEL // 4
    inv_freq = jnp.exp(-math.log(10000.0) * jnp.arange(quarter, dtype=F32) / quarter)

    def emb(pos):
        ang = pos.reshape(-1)[:, None] * inv_freq[None, :]
        return jnp.concatenate([jnp.sin(ang), jnp.cos(ang)], axis=-1)

    return jnp.concatenate([emb(rr), emb(cc)], axis=-1).astype(dtype)


def segsum(a):
    t = a.shape[-1]
    rep = jnp.broadcast_to(a[..., :, None], a.shape + (t,))
    strict = jnp.tril(jnp.ones((t, t), dtype=bool), -1)
    cs = jnp.cumsum(jnp.where(strict, rep, 0.0), axis=-2)
    return jnp.where(jnp.tril(jnp.ones((t, t), dtype=bool)), cs, -jnp.inf)


def ssd_chunked_scan(xdt, a, bm, cm, h0):
    bsz, n, nh, hp = xdt.shape
    nc = n // SSD_CHUNK
    X = xdt.reshape(bsz, nc, SSD_CHUNK, nh, hp)
    A = a.reshape(bsz, nc, SSD_CHUNK, nh).transpose(0, 3, 1, 2)
    Bc = bm.reshape(bsz, nc, SSD_CHUNK, nh, -1)
    Cc = cm.reshape(bsz, nc, SSD_CHUNK, nh, -1)
    a_cs = jnp.cumsum(A, axis=-1)
    scores = jnp.einsum("bclhn,bcshn->bhcls", Cc, Bc) * jnp.exp(segsum(A))
    y_diag = jnp.einsum("bhcls,bcshp->bclhp", scores, X)
    decay_states = jnp.exp(a_cs[..., -1:] - a_cs)
    states = jnp.einsum("bclhn,bhcl,bclhp->bchpn", Bc, decay_states, X)
    states = jnp.concatenate([h0[:, None], states], axis=1)
    chunk_tot = jnp.pad(a_cs[..., -1], ((0, 0), (0, 0), (1, 0)))
    new_states = jnp.einsum("bhzc,bchpn->bzhpn", jnp.exp(segsum(chunk_tot)), states)
    states_in, final = new_states[:, :-1], new_states[:, -1]
    y_off = jnp.einsum("bclhn,bchpn,bhcl->bclhp", Cc, states_in, jnp.exp(a_cs))
    return (y_diag + y_off).reshape(bsz, n, nh, hp), final


def ssd_mixer(cols, conv_w, conv_b, dt_bias, a_log, d_skip, norm_g, h0):
    bsz, n, _ = cols.shape
    z = cols[..., :SSD_WIDTH]
    xbc = cols[..., SSD_WIDTH:SSD_WIDTH + SSD_XBC]
    dt_raw = cols[..., SSD_WIDTH + SSD_XBC:]
    xbc = jax.nn.silu(depthwise_conv(xbc, conv_w, conv_b)).astype(F32)
    gn = SSD_GROUPS * SSD_STATE
    rep = SSD_HEADS // SSD_GROUPS
    xs = xbc[..., :SSD_WIDTH].reshape(bsz, n, SSD_HEADS, SSD_HEAD_DIM)
    bm = jnp.repeat(xbc[..., SSD_WIDTH:SSD_WIDTH + gn].reshape(bsz, n, SSD_GROUPS, SSD_STATE), rep, axis=2)
    cm = jnp.repeat(xbc[..., SSD_WIDTH + gn:].reshape(bsz, n, SSD_GROUPS, SSD_STATE), rep, axis=2)
    dt = jax.nn.softplus(dt_raw.astype(F32).reshape(bsz, n, 2, SSD_HEADS) + dt_bias.astype(F32))
    a = -jnp.exp(a_log.astype(F32))
    y = d_skip.astype(F32)[:, None] * xs
    finals = []
    for d in range(2):
        dt_d = dt[:, :, d]
        args = (xs * dt_d[..., None], dt_d * a[d], bm, cm)
        if d == 1:
            args = tuple(jnp.flip(t, axis=1) for t in args)
        y_d, fin = ssd_chunked_scan(*args, h0[d])
        y = y + (y_d if d == 0 else jnp.flip(y_d, axis=1))
        finals.append(fin)
    y = y.reshape(bsz, n, SSD_WIDTH) * jax.nn.silu(z.astype(F32))
    return rms_norm(y, norm_g).astype(cols.dtype), finals


def conformer_conv(cols, dw_w, dw_b, ln_g, ln_b, pw_w, pw_b):
    u = cols[..., :CONF_WIDTH] * jax.nn.sigmoid(cols[..., CONF_WIDTH:])
    u = depthwise_conv(u, dw_w, dw_b)
    u = jax.nn.silu(layer_norm(u, ln_g, ln_b))
    return u @ pw_w + pw_b


def _complex_linear_combine(e1, e2):
    a1r, a1i, b1r, b1i = e1
    a2r, a2i, b2r, b2i = e2
    return (a2r * a1r - a2i * a1i,
            a2r * a1i + a2i * a1r,
            a2r * b1r - a2i * b1i + b2r,
            a2r * b1i + a2i * b1r + b2i)


def s5_mixer(u, lam_re, lam_im, log_step, b_re, b_im, c_re, c_im, d_skip, glu_w, glu_b, h0):
    bsz, n, _ = u.shape
    u32 = u.astype(F32)
    ug = u32.reshape(bsz, n, S5_GROUPS, S5_GROUP_CH)
    lam_re, lam_im = lam_re.astype(F32), lam_im.astype(F32)
    b_re, b_im, c_re, c_im = (t.astype(F32) for t in (b_re, b_im, c_re, c_im))
    y = jnp.zeros((bsz, n, S5_GROUPS, S5_GROUP_CH), F32)
    finals = []
    for d in range(2):
        step = jnp.exp(log_step[d].astype(F32))[:, None]
        lr, li = lam_re[d], lam_im[d]
        mag = jnp.exp(lr * step)
        ar, ai = mag * jnp.cos(li * step), mag * jnp.sin(li * step)
        inv_den = 1.0 / (lr * lr + li * li)
        cr = ((ar - 1.0) * lr + ai * li) * inv_den
        ci = (ai * lr - (ar - 1.0) * li) * inv_den
        bbr = cr[..., None] * b_re - ci[..., None] * b_im
        bbi = cr[..., None] * b_im + ci[..., None] * b_re
        src = ug if d == 0 else jnp.flip(ug, axis=1)
        bur = jnp.einsum("blgh,gph->blgp", src, bbr)
        bui = jnp.einsum("blgh,gph->blgp", src, bbi)
        h0r, h0i = h0[d]
        bur = bur.at[:, 0].add(ar * h0r - ai * h0i)
        bui = bui.at[:, 0].add(ar * h0i + ai * h0r)
        _, _, hr, hi = lax.associative_scan(
            _complex_linear_combine,
            (jnp.broadcast_to(ar, bur.shape), jnp.broadcast_to(ai, bur.shape), bur, bui), axis=1)
        finals.append((hr[:, -1], hi[:, -1]))
        yd = jnp.einsum("blgp,ghp->blgh", hr, c_re) - jnp.einsum("blgp,ghp->blgh", hi, c_im)
        y = y + (yd if d == 0 else jnp.flip(yd, axis=1))
    y = jax.nn.gelu(y.reshape(bsz, n, S5_WIDTH) + d_skip.astype(F32) * u32)
    gv = y @ glu_w.astype(F32) + glu_b.astype(F32)
    out = gv[..., :S5_WIDTH] * jax.nn.sigmoid(gv[..., S5_WIDTH:])
    return out.astype(u.dtype), finals


def moe_ffn(h, router_w, router_b, w_gate, w_up, w_down):
    n_tok = h.shape[0]
    logits = jnp.matmul(h, router_w).astype(F32) + router_b.astype(F32)
    probs = jax.nn.softmax(logits, axis=-1)
    grp = probs.reshape(n_tok, N_EXPERT_GROUPS, EXPERTS_PER_GROUP)
    group_score = jnp.sum(lax.top_k(grp, TOP_K)[0], axis=-1)
    best = jnp.argmax(group_score, axis=-1)
    in_group = (jnp.arange(N_EXPERTS) // EXPERTS_PER_GROUP)[None, :] == best[:, None]
    vals, idx = lax.top_k(jnp.where(in_group, probs, -jnp.inf), TOP_K)
    gates = vals / jnp.sum(vals, axis=-1, keepdims=True)
    dense_gate = jnp.sum(jax.nn.one_hot(idx, N_EXPERTS, dtype=F32) * gates[..., None], axis=1).astype(h.dtype)
    out = jnp.zeros_like(h)
    for e in range(N_EXPERTS):
        act = jax.nn.silu(h @ w_gate[e]) * (h @ w_up[e])
        out = out + dense_gate[:, e:e + 1] * (act @ w_down[e])
    return out


def hybrid_layer(x, ctx, mod_lat, mod_ctx, p, router_w, router_b, with_ctx_out):
    sh1, sc1, g1, sh2, sc2, g2 = jnp.split(mod_lat, N_MOD, axis=-1)
    csh1, csc1, cg1, csh2, csc2, cg2 = jnp.split(mod_ctx, N_MOD, axis=-1)
    bsz = x.shape[0]
    h = rms_norm(x, p["g_mix"]) * (1 + sc1) + sh1
    hc = rms_norm(ctx, p["g_mix"]) * (1 + csc1) + csh1
    cols = h @ p["w_in"]
    ccols = hc @ p["w_in"]
    o_b, o_c = SSD_COLS, SSD_COLS + CONF_COLS

    ssd_args = (p["ssd_conv_w"], p["ssd_conv_b"], p["ssd_dt_bias"], p["ssd_a_log"], p["ssd_d"], p["ssd_norm_g"])
    zero_ssd = jnp.zeros((bsz, SSD_HEADS, SSD_HEAD_DIM, SSD_STATE), F32)
    a_ctx, ssd_states = ssd_mixer(ccols[..., :o_b], *ssd_args, [zero_ssd, zero_ssd])
    a_lat, _ = ssd_mixer(cols[..., :o_b], *ssd_args, ssd_states)

    s5_args = (p["s5_lambda_re"], p["s5_lambda_im"], p["s5_log_step"], p["s5_b_re"], p["s5_b_im"],
               p["s5_c_re"], p["s5_c_im"], p["s5_d"], p["s5_glu_w"], p["s5_glu_b"])
    zero_s5 = (jnp.zeros((bsz, S5_GROUPS, S5_STATE), F32), jnp.zeros((bsz, S5_GROUPS, S5_STATE), F32))
    s_ctx, s5_states = s5_mixer(ccols[..., o_c:], *s5_args, [zero_s5, zero_s5])
    s_lat, _ = s5_mixer(cols[..., o_c:], *s5_args, s5_states)

    conf_args = (p["conf_dw_w"], p["conf_dw_b"], p["conf_ln_g"], p["conf_ln_b"], p["conf_pw_w"], p["conf_pw_b"])
    b_lat = conformer_conv(cols[..., o_b:o_c], *conf_args)

    mix = jnp.concatenate([a_lat, b_lat.astype(x.dtype), s_lat], axis=-1)
    x = x + g1 * (mix @ p["w_out"])
    h2 = rms_norm(x, p["g_ffn"]) * (1 + sc2) + sh2
    moe_w = (p["exp_w_gate"], p["exp_w_up"], p["exp_w_down"])
    if with_ctx_out:
        b_ctx = conformer_conv(ccols[..., o_b:o_c], *conf_args)
        cmix = jnp.concatenate([a_ctx, b_ctx.astype(ctx.dtype), s_ctx], axis=-1)
        ctx = ctx + cg1 * (cmix @ p["w_out"])
        hc2 = rms_norm(ctx, p["g_ffn"]) * (1 + csc2) + csh2
        n_lat = h2.shape[0] * h2.shape[1]
        tokens = jnp.concatenate([h2.reshape(-1, D_MODEL), hc2.reshape(-1, D_MODEL)], axis=0)
        ff = moe_ffn(tokens, router_w, router_b, *moe_w)
        x = x + g2 * ff[:n_lat].reshape(x.shape)
        ctx = ctx + cg2 * ff[n_lat:].reshape(ctx.shape)
    else:
        x = x + g2 * moe_ffn(h2.reshape(-1, D_MODEL), router_w, router_b, *moe_w).reshape(x.shape)
    return x, ctx


def setup_inputs(seed: int = 0) -> dict:
    key = jax.random.key(seed)
    ks = iter(jax.random.split(key, 64))
    L = DEPTH

    def nrm(shape, scale):
        return jax.random.normal(next(ks), shape, F32) * scale

    def gain(shape):
        return 1.0 + nrm(shape, 0.01)

    def unif(shape, lo, hi):
        return jax.random.uniform(next(ks), shape, F32, minval=lo, maxval=hi)

    dt0 = jnp.exp(unif((L, 2, SSD_HEADS), math.log(1e-3), math.log(1e-1)))
    s5_n = jnp.arange(S5_STATE, dtype=F32)
    return {
        "x": nrm((BATCH, SEQ, D_MODEL), 1.0),
        "c": nrm((BATCH, D_MODEL), 1.0),
        "ctx": nrm((BATCH, CTX_LEN, D_MODEL), 1.0),
        "c_ctx": nrm((D_MODEL,), 1.0),
        "w_ada": nrm((L, D_MODEL, N_MOD * D_MODEL), 0.5 * D_MODEL ** -0.5),
        "b_ada": nrm((L, N_MOD * D_MODEL), 0.01),
        "g_mix": gain((L, D_MODEL)),
        "w_in": nrm((L, D_MODEL, IN_COLS), D_MODEL ** -0.5),
        "ssd_conv_w": nrm((L, SSD_CONV, SSD_XBC), SSD_CONV ** -0.5),
        "ssd_conv_b": nrm((L, SSD_XBC), 0.01),
        "ssd_dt_bias": dt0 + jnp.log(-jnp.expm1(-dt0)),
        "ssd_a_log": jnp.log(unif((L, 2, SSD_HEADS), 1.0, 16.0)),
        "ssd_d": gain((L, SSD_HEADS)),
        "ssd_norm_g": gain((L, SSD_WIDTH)),
        "conf_dw_w": nrm((L, CONF_KERNEL, CONF_WIDTH), CONF_KERNEL ** -0.5),
        "conf_dw_b": nrm((L, CONF_WIDTH), 0.01),
        "conf_ln_g": gain((L, CONF_WIDTH)),
        "conf_ln_b": nrm((L, CONF_WIDTH), 0.01),
        "conf_pw_w": nrm((L, CONF_WIDTH, CONF_WIDTH), CONF_WIDTH ** -0.5),
        "conf_pw_b": nrm((L, CONF_WIDTH), 0.01),
        "s5_lambda_re": -0.5 + nrm((L, 2, S5_GROUPS, S5_STATE), 0.01),
        "s5_lambda_im": math.pi * s5_n + nrm((L, 2, S5_GROUPS, S5_STATE), 0.01),
        "s5_log_step": unif((L, 2, S5_GROUPS), math.log(1e-3), math.log(1e-1)),
        "s5_b_re": nrm((L, S5_GROUPS, S5_STATE, S5_GROUP_CH), (2 * S5_GROUP_CH) ** -0.5),
        "s5_b_im": nrm((L, S5_GROUPS, S5_STATE, S5_GROUP_CH), (2 * S5_GROUP_CH) ** -0.5),
        "s5_c_re": nrm((L, S5_GROUPS, S5_GROUP_CH, S5_STATE), (2 * S5_STATE) ** -0.5),
        "s5_c_im": nrm((L, S5_GROUPS, S5_GROUP_CH, S5_STATE), (2 * S5_STATE) ** -0.5),
        "s5_d": nrm((L, S5_WIDTH), 1.0),
        "s5_glu_w": nrm((L, S5_WIDTH, 2 * S5_WIDTH), S5_WIDTH ** -0.5),
        "s5_glu_b": nrm((L, 2 * S5_WIDTH), 0.01),
        "w_out": nrm((L, MIX_WIDTH, D_MODEL), MIX_WIDTH ** -0.5),
        "g_ffn": gain((L, D_MODEL)),
        "router_w": nrm((D_MODEL, N_EXPERTS), D_MODEL ** -0.5),
        "router_b": nrm((N_EXPERTS,), 0.01),
        "exp_w_gate": nrm((L, N_EXPERTS, D_MODEL, EXPERT_FF), D_MODEL ** -0.5),
        "exp_w_up": nrm((L, N_EXPERTS, D_MODEL, EXPERT_FF), D_MODEL ** -0.5),
        "exp_w_down": nrm((L, N_EXPERTS, EXPERT_FF, D_MODEL), EXPERT_FF ** -0.5),
        "g_final": gain((D_MODEL,)),
    }


def reference(x, c, ctx, c_ctx, w_ada, b_ada, g_mix, w_in, ssd_conv_w, ssd_conv_b, ssd_dt_bias,
              ssd_a_log, ssd_d, ssd_norm_g, conf_dw_w, conf_dw_b, conf_ln_g, conf_ln_b, conf_pw_w,
              conf_pw_b, s5_lambda_re, s5_lambda_im, s5_log_step, s5_b_re, s5_b_im, s5_c_re, s5_c_im,
              s5_d, s5_glu_w, s5_glu_b, w_out, g_ffn, router_w, router_b, exp_w_gate, exp_w_up,
              exp_w_down, g_final):
    rows = x.shape[1] // GRID_W
    x = x + grid_pos_embed(rows, x.dtype)[None]
    cond_lat = jax.nn.silu(c)
    cond_ctx = jax.nn.silu(c_ctx)
    for l in range(DEPTH):
        mod_lat = (cond_lat @ w_ada[l] + b_ada[l])[:, None, :]
        mod_ctx = (cond_ctx @ w_ada[l] + b_ada[l])[None, None, :]
        p = dict(
            g_mix=g_mix[l], w_in=w_in[l], ssd_conv_w=ssd_conv_w[l], ssd_conv_b=ssd_conv_b[l],
            ssd_dt_bias=ssd_dt_bias[l], ssd_a_log=ssd_a_log[l], ssd_d=ssd_d[l], ssd_norm_g=ssd_norm_g[l],
            conf_dw_w=conf_dw_w[l], conf_dw_b=conf_dw_b[l], conf_ln_g=conf_ln_g[l], conf_ln_b=conf_ln_b[l],
            conf_pw_w=conf_pw_w[l], conf_pw_b=conf_pw_b[l], s5_lambda_re=s5_lambda_re[l],
            s5_lambda_im=s5_lambda_im[l], s5_log_step=s5_log_step[l], s5_b_re=s5_b_re[l],
            s5_b_im=s5_b_im[l], s5_c_re=s5_c_re[l], s5_c_im=s5_c_im[l], s5_d=s5_d[l],
            s5_glu_w=s5_glu_w[l], s5_glu_b=s5_glu_b[l], w_out=w_out[l], g_ffn=g_ffn[l],
            exp_w_gate=exp_w_gate[l], exp_w_up=exp_w_up[l], exp_w_down=exp_w_down[l])
        x, ctx = hybrid_layer(x, ctx, mod_lat, mod_ctx, p, router_w, router_b, l < DEPTH - 1)
    return rms_norm(x, g_final)
```

```python
import contextlib
import math
import numpy as np
import concourse.bass as bass
import concourse.mybir as mybir
from concourse.bass_utils import run_bass_kernel_spmd

F32 = mybir.dt.float32
BF16 = mybir.dt.bfloat16
ALU = mybir.AluOpType
AF = mybir.ActivationFunctionType

NL = 2
T = 2304
TC = 256
NCH = 18
EPS = 1e-6
SQ = 8
NSC = T // SQ
NLEV = 9

PV = {}
_o = 0
for _n, _w in (("b_ada", 48), ("g_mix", 8), ("g_ffn", 8), ("conv_w", 40), ("conv_b", 8), ("ssd_d", 4),
               ("ssd_ng", 4), ("dw_w", 62), ("dw_b", 2), ("ln_g", 2), ("ln_b", 2), ("pw_b", 2), ("s5_d", 2),
               ("glu_b", 4), ("g_final", 8), ("lamre", 16), ("lamim", 16), ("lstep", 16),
               ("bre", 128), ("bim", 128), ("cre", 128), ("cim", 128)):
    PV[_n] = _o
    _o += _w
NPV = _o
NPB = 48
WIN_TILES = [(i * 128, 128) for i in range(12)] + [(1536, 16)] + [(1552 + i * 128, 128) for i in range(4)] + \
            [(2064 + i * 128, 128) for i in range(2)]
J_Z, J_XBC, J_DT, J_CONF, J_S5 = 0, 4, 12, 13, 17

NTILES = [(0, 256)] + [(256 + 512 * i, 256 + 512 * (i + 1)) for i in range(4)]


class Tracker:
    def __init__(self, nc, es):
        self.nc = nc
        self.es = es
        self.eng = dict(pe=nc.tensor, dve=nc.vector, act=nc.scalar, pool=nc.gpsimd, sp=nc.sync)
        self.semh = {}
        self.cnt = {}
        self.seen = {e: {} for e in self.eng}
        self.st = {}
        self.nins = 0

    def sem(self, name):
        if name not in self.semh:
            self.semh[name] = self.es.enter_context(self.nc.semaphore(name))
            self.cnt[name] = 0
        return self.semh[name]

    def _wait(self, e, toks):
        eng = self.eng[e]
        seen = self.seen[e]
        for (s, v) in toks:
            if seen.get(s, 0) < v:
                eng.wait_ge(self.semh[s], v)
                seen[s] = v

    def _deps(self, r, w):
        toks = []
        for k in r:
            st = self.st.get(k)
            if st and st[0]:
                toks.append(st[0])
        for k in w:
            st = self.st.get(k)
            if st:
                if st[0]:
                    toks.append(st[0])
                toks.extend(st[1].items())
        return toks

    def _record(self, tok, r, w):
        for k in r:
            st = self.st.setdefault(k, [None, {}])
            if st[1].get(tok[0], 0) < tok[1]:
                st[1][tok[0]] = tok[1]
        for k in w:
            self.st[k] = [tok, {}]

    def op(self, e, fns, r=(), w=()):
        self._wait(e, self._deps(r, w))
        if callable(fns):
            fns = [fns]
        ins = None
        for f in fns:
            ins = f()
            self.nins += 1
        s = "s_" + e
        self.sem(s)
        ins.then_inc(self.semh[s], 1)
        self.cnt[s] += 1
        tok = (s, self.cnt[s])
        self._record(tok, r, w)
        return tok

    def dma(self, q, out, in_, r=(), w=(), sem=None):
        self._wait(q, self._deps(r, w))
        s = sem or ("d_" + (w[0] if w else r[0]))
        self.sem(s)
        self.eng[q].dma_start(out=out, in_=in_).then_inc(self.semh[s], 16)
        self.cnt[s] += 16
        tok = (s, self.cnt[s])
        self._record(tok, r, w)
        self.nins += 1
        return tok

    def barrier(self):
        toks = [(s, v) for s, v in self.cnt.items() if v > 0]
        for e in self.eng:
            self._wait(e, toks)
        self.st = {}


class Builder:
    def __init__(self, nc, es, dbg=None, stop=None, nlayers=NL):
        self.nc = nc
        self.es = es
        self.t = Tracker(nc, es)
        self.dbg = dbg or []
        self.stop = stop
        self.nlayers = nlayers
        self.AW = 53200
        self.arena = es.enter_context(nc.sbuf_tensor("arena", [128, self.AW], F32))
        self.ps = [es.enter_context(nc.psum_tensor(f"psb{i}", [128, 512], F32)) for i in range(8)]
        self.dumps = []
        self.uid = 0

    def region(self, start_words, nwords):
        return [start_words, start_words, start_words + nwords]

    def alloc(self, reg, n, dtype=F32):
        words = n if dtype == F32 else (n + 1) // 2
        words = (words + 7) // 8 * 8
        o = reg[1]
        assert o + words <= reg[2], f"arena region overflow: need {words} have {reg[2] - o}"
        reg[1] = o + words
        ap = self.arena[:, o:o + (n if dtype == F32 else (n + 1) // 2)]
        if dtype != F32:
            ap = ap.bitcast(dtype)
        return ap

    def key(self, base):
        self.uid += 1
        return f"{base}#{self.uid}"

    def V(self, name, r, w, **kw):
        return self.t.op("dve", lambda: getattr(self.nc.vector, name)(**kw), r, w)

    def A(self, r, w, **kw):
        return self.t.op("act", lambda: self.nc.scalar.activation(**kw), r, w)

    def G(self, name, r, w, **kw):
        return self.t.op("pool", lambda: getattr(self.nc.gpsimd, name)(**kw), r, w)

    def MM(self, mms, r, w):
        fns = [(lambda kw=kw: self.nc.tensor.matmul(kw["out"], lhsT=kw["lhsT"], rhs=kw["rhs"],
                                                    start=kw.get("start", True), stop=kw.get("stop", True)))
               for kw in mms]
        return self.t.op("pe", fns, r, w)

    def TR(self, trs, r, w):
        fns = [(lambda kw=kw: self.nc.tensor.transpose(kw["out"], kw["in_"], kw["ident"])) for kw in trs]
        return self.t.op("pe", fns, r, w)

    def wexp_ap(self):
        if not hasattr(self, "_wexp"):
            self._wexp = self.nc.dram_tensor("wexp", [NL * 32, 128, 3 * 4096], F32, kind="ExternalInput").ap()
            self.uses_moe = True
        return self._wexp

    def dump(self, name, ap2d, n, r):
        d = self.nc.dram_tensor("dbg_" + name, [128, n], F32, kind="ExternalOutput").ap()
        self.t.dma("sp", d, ap2d, r=r, w=[], sem="d_out")

    def dump_any(self, name, ap2d, n, r, dtype):
        if name not in self.dbg:
            return
        if dtype == F32:
            self.dump(name, ap2d, n, r)
        else:
            tmp = self.alloc(self.regD, n, F32)
            k = self.key("dbgtmp")
            self.V("tensor_copy", r, [k], out=tmp, in_=ap2d)
            self.dump(name, tmp, n, [k])


def v3(ap, a):
    return ap.rearrange("p (a b) -> p a b", a=a)


def v4(ap, a, b):
    return ap.rearrange("p (a b c) -> p a b c", a=a, b=b)


def build(nc, es, dbg=None, stop=None, nlayers=NL):
    B = Builder(nc, es, dbg, stop, nlayers)
    t = B.t
    V, A, G, MM, TR = B.V, B.A, B.G, B.MM, B.TR
    ps = B.ps

    def din(name, shape):
        return nc.dram_tensor(name, shape, F32, kind="ExternalInput").ap()

    xin = din("xin", [128, 8 * T])
    cond = din("cond", [128, 16])
    wada = din("wada", [NL * 12, 128, 8 * 512])
    pvec_d = din("pvec", [128, NL * NPV])
    pbc_d = din("pbc", [128, NL * NPB])
    consts_d = din("consts", [128, 6 * 128])
    win = din("win", [NL * 19, 128, 8 * 128])
    rw_d = din("rw", [128, 8 * 16])
    wpw_d = din("wpw", [NL, 128, 2 * 256])
    wglu_d = din("wglu", [NL, 128, 2 * 512])
    wout_d = din("wout", [NL, 128, 8 * 1024])
    out_d = nc.dram_tensor("out", [128, 8 * 2048], F32, kind="ExternalOutput").ap()
    xsp = nc.dram_tensor("xsp", [128, 8 * T], F32, kind="Internal").ap()

    P0 = B.region(0, 4608)
    HT0 = 4608
    HTW = 8 * T // 2
    R0 = HT0 + HTW
    AW = 8 * T
    MW = 4 * T // 2
    regA = B.region(R0, AW)
    regM = B.region(R0 + AW, MW)
    regB = B.region(R0 + AW + MW, B.AW - (R0 + AW + MW))
    regMB = B.region(R0 + AW, B.AW - (R0 + AW))
    B.regD = B.region(0, 0)

    def reset(reg):
        reg[1] = reg[0]

    hT = B.arena[:, HT0:HT0 + HTW].bitcast(BF16)
    hT3 = v3(hT, 8)
    xT = B.arena[:, R0:R0 + AW]
    x3 = v3(xT, 8)
    mixbs = B.arena[:, R0 + AW:R0 + AW + MW].bitcast(BF16)
    mix3 = v3(mixbs, 4)

    consts = B.alloc(P0, 6 * 128)
    ident_f, ones_f, LE_f, GT_f, GE_f, LT_f = [consts[:, i * 128:(i + 1) * 128] for i in range(6)]
    cb = B.alloc(P0, 2 * 128, BF16)
    ident_b, ones_b = cb[:, 0:128], cb[:, 128:256]
    pvec = B.alloc(P0, NL * NPV)
    pbc = B.alloc(P0, NL * NPB)
    condS = B.alloc(P0, 16)
    modT = B.alloc(P0, NL * 96)
    der = B.alloc(P0, NL * 32)
    rwf = B.alloc(P0, 128)

    def pv(l, name, c=0, n=1):
        o = l * NPV + PV[name] + c
        return pvec[:, o:o + n]

    def modc(l, j, col):
        o = l * 96 + j * 2 + col
        return modT[:, o:o + 1]

    def derc(l, which, k, col):
        o = l * 32 + which * 16 + k * 2 + col
        return der[:, o:o + 1]

    t.dma("sp", consts, consts_d, w=["consts"])
    t.dma("sp", pvec, pvec_d, w=["pvec"])
    t.dma("sp", pbc, pbc_d, w=["pbc"])
    t.dma("sp", condS, cond, w=["cond"])
    t.dma("sp", rwf, rw_d, w=["rwf"])
    V("tensor_copy", ["consts"], ["cb"], out=cb, in_=consts[:, 0:256])
    A(["cond"], ["condS"], out=condS, in_=condS, func=AF.Silu)
    for k in range(8):
        t.dma("sp", x3[:, k, :], xin[:, k * T:(k + 1) * T], w=[f"xT{k}"])
    reset(regB)
    pidx = B.alloc(regB, 8)
    invf = B.alloc(regB, 8)
    iof = B.alloc(regB, 64)
    ang = B.alloc(regB, 128)
    sn_, cs__, cc_, ss_ = [B.alloc(regB, 128) for _ in range(4)]
    PE_ = "posemb"
    V("reduce_sum", ["consts"], [PE_], out=pidx[:, 0:1], in_=LE_f, axis=mybir.AxisListType.X)
    V("tensor_scalar", [PE_], [PE_], out=pidx[:, 0:1], in0=pidx[:, 0:1], scalar1=-1.0, scalar2=128.0, op0=ALU.mult, op1=ALU.add)
    V("tensor_scalar", [PE_], [PE_], out=pidx[:, 1:2], in0=pidx[:, 0:1], scalar1=128.0, scalar2=None, op0=ALU.add)
    A([PE_], [PE_], out=invf[:, 0:2], in_=pidx[:, 0:2], func=AF.Exp, scale=-math.log(10000.0) / 256.0)
    MM([dict(out=ps[3][:, 0:128], lhsT=ones_f, rhs=LT_f)], ["consts"], ["ps3"])
    V("tensor_copy", ["ps3"], [PE_], out=iof, in_=ps[3][:, 0:64])
    V("tensor_tensor", [PE_], [PE_], out=v3(ang, 2), in0=iof.unsqueeze(1).broadcast_to([128, 2, 64]),
      in1=invf[:, 0:2].unsqueeze(2).broadcast_to([128, 2, 64]), op=ALU.mult)
    A([PE_], [PE_], out=sn_, in_=ang, func=AF.Sin, scale=1.0 / 64.0)
    A([PE_], [PE_], out=cs__, in_=ang, func=AF.Sin, scale=1.0 / 64.0, bias=math.pi / 2)
    for _ in range(6):
        V("tensor_tensor", [PE_], [PE_], out=cc_, in0=cs__, in1=cs__, op=ALU.mult)
        V("tensor_tensor", [PE_], [PE_], out=ss_, in0=sn_, in1=sn_, op=ALU.mult)
        V("scalar_tensor_tensor", [PE_], [PE_], out=sn_, in0=cs__, scalar=2.0, in1=sn_, op0=ALU.mult, op1=ALU.mult)
        V("tensor_tensor", [PE_], [PE_], out=cs__, in0=cc_, in1=ss_, op=ALU.subtract)
    for k in range(8):
        tab = v3(sn_ if (k // 2) % 2 == 0 else cs__, 2)[:, k % 2, :]
        xl = x3[:, k, TC:].rearrange("p (r c) -> p r c", c=64)
        if k < 4:
            in1 = tab[:, 0:32].unsqueeze(2).broadcast_to([128, 32, 64])
        else:
            in1 = tab.unsqueeze(1).broadcast_to([128, 32, 64])
        V("tensor_tensor", [PE_, f"xT{k}"], [f"xT{k}"], out=xl, in0=xl, in1=in1, op=ALU.add)
    t.barrier()

    reset(regMB)
    wslots = [B.alloc(regMB, 8 * 512) for _ in range(2)]
    modrow = B.alloc(regMB, 6144)
    condS3 = v3(condS, 8)
    gi = 0
    for l in range(nlayers):
        for blk in range(12):
            s = gi % 2
            gi += 1
            t.dma("sp", wslots[s], wada[l * 12 + blk], w=[f"wada{s}"], sem=f"d_wada{s}")
            pi = 1 + s
            MM([dict(out=ps[pi][0:2, 0:512], lhsT=condS3[:, kc, :], rhs=wslots[s][:, kc * 512:(kc + 1) * 512],
                     start=(kc == 0), stop=(kc == 7)) for kc in range(8)], [f"wada{s}", "condS"], [f"ps{pi}"])
            A([f"ps{pi}"], ["modrow"], out=modrow[0:2, blk * 512:(blk + 1) * 512], in_=ps[pi][0:2, 0:512], func=AF.Copy)
        TR([dict(out=ps[0][:, l * 96 + j * 2: l * 96 + j * 2 + 2], in_=modrow[0:2, j * 128:(j + 1) * 128], ident=ident_f[0:2, 0:2])
            for j in range(48)], ["modrow", "consts"], ["ps0"])
        V("tensor_tensor", ["ps0", "pvec"], ["modT"], out=v3(modT[:, l * 96:(l + 1) * 96], 48),
          in0=v3(ps[0][:, l * 96:(l + 1) * 96], 48),
          in1=pv(l, "b_ada", 0, 48).unsqueeze(2).broadcast_to([128, 48, 2]), op=ALU.add)
        for which, (gname, scj) in enumerate((("g_mix", 8), ("g_ffn", 32))):
            dst = v3(der[:, l * 32 + which * 16: l * 32 + which * 16 + 16], 8)
            V("tensor_scalar", ["modT"], ["der"], out=dst, in0=v3(modT[:, l * 96 + scj * 2: l * 96 + scj * 2 + 16], 8),
              scalar1=1.0, scalar2=32.0, op0=ALU.add, op1=ALU.mult)
            V("tensor_tensor", ["der", "pvec"], ["der"], out=dst, in0=dst,
              in1=pv(l, gname, 0, 8).unsqueeze(2).broadcast_to([128, 8, 2]), op=ALU.mult)
    t.barrier()
    if "mod" in B.dbg:
        B.dump("mod", modT, NL * 96, ["modT"])
        B.dump("x0", xT, 8 * T, [f"xT{k}" for k in range(8)])

    XK = [f"xT{k}" for k in range(8)]

    def rmsnorm_to_h(l, which, reg, tok_ssq=None, ntiles=NTILES):
        shj = 0 if which == 0 else 24
        sqb = B.alloc(reg, 8 * 512, BF16)
        sqb3 = v3(sqb, 8)
        rs = [B.alloc(reg, 512) for _ in range(2)]
        yt = [B.alloc(reg, 512) for _ in range(2)]
        it = 0
        for ti, (n0, n1) in enumerate(ntiles):
            n = n1 - n0
            col = 1 if n0 < TC else 0
            for k in range(8):
                A([f"xT{k}"], [f"sqb{k}"], out=sqb3[:, k, 0:n], in_=x3[:, k, n0:n1], func=AF.Square)
            MM([dict(out=ps[1][:, 0:n], lhsT=ones_b, rhs=sqb3[:, k, 0:n], start=(k == 0), stop=(k == 7)) for k in range(8)],
               [f"sqb{k}" for k in range(8)] + ["cb"], ["ps1"])
            if tok_ssq is not None:
                for c in range(n // 128):
                    ch = (n0 + c * 128) // 128
                    MM([dict(out=ps[2][:, ch:ch + 1], lhsT=sqb3[:, k, c * 128:(c + 1) * 128], rhs=ones_b[:, 0:1],
                             start=(k == 0), stop=(k == 7)) for k in range(8)],
                       [f"sqb{k}" for k in range(8)] + ["cb"], ["ps2"])
            r_ = rs[ti % 2]
            rk = f"rs{ti % 2}"
            A(["ps1"], [rk], out=r_[:, 0:n], in_=ps[1][:, 0:n], func=AF.Sqrt, bias=1024.0 * EPS, scale=1.0)
            V("reciprocal", [rk], [rk], out=r_[:, 0:n], in_=r_[:, 0:n])
            for k in range(8):
                y_ = yt[it % 2]
                yk = f"yt{it % 2}"
                it += 1
                V("tensor_tensor", [f"xT{k}", rk], [yk], out=y_[:, 0:n], in0=x3[:, k, n0:n1], in1=r_[:, 0:n], op=ALU.mult)
                A([yk, "der", "modT"], [f"hT{k}"], out=hT3[:, k, n0:n1], in_=y_[:, 0:n], func=AF.Identity,
                  scale=derc(l, which, k, col), bias=modc(l, shj + k, col))
        if tok_ssq is not None:
            V("tensor_copy", ["ps2"], ["tokssq"], out=tok_ssq, in_=ps[2][:, 0:NCH])

    HK = [f"hT{k}" for k in range(8)]

    win_state = {"i": 0}

    def load_win_tile(l, j, reg_slots):
        s = win_state["i"] % len(reg_slots)
        win_state["i"] += 1
        t.dma("pool", reg_slots[s], win[l * 19 + j], w=[f"wins{s}"], sem=f"d_wins{s}")
        return v3(reg_slots[s], 8), f"wins{s}"

    def proj(wt, wk, n0, n1, pst, psk, m=128):
        MM([dict(out=pst[0:m, 0:n1 - n0], lhsT=wt[:, kc, 0:m], rhs=hT3[:, kc, n0:n1], start=(kc == 0), stop=(kc == 7))
            for kc in range(8)], [wk] + HK, [psk])

    for l in range(nlayers):
        last = (l == NL - 1)
        NT_l = NTILES[1:] if last else NTILES
        reset(regB)
        rmsnorm_to_h(l, 0, regB)
        for k in range(8):
            t.dma("sp", xsp[:, k * T:(k + 1) * T], x3[:, k, :], r=[f"xT{k}"], w=[f"xsp{k}"])
        if "h" in B.dbg and l == 0:
            reset(regA)
            B.regD = regA
            t.barrier()
            B.dump_any("h", hT, 8 * T, HK, BF16)
        t.barrier()
        if stop == "h":
            break

        reset(regA)
        reset(regB)
        wslots_in = [B.alloc(regB, 8 * 128, BF16) for _ in range(3)]
        uTb = B.alloc(regB, 2 * T, BF16)
        uT3 = v3(uTb, 2)
        for c in range(2):
            wt, wk = load_win_tile(l, J_S5 + c, wslots_in)
            for ti, (n0, n1) in enumerate(NTILES):
                pi = 3 + (ti % 2)
                proj(wt, wk, n0, n1, ps[pi], f"ps{pi}")
                A([f"ps{pi}"], [f"uT{c}"], out=uT3[:, c, n0:n1], in_=ps[pi][:, 0:n1 - n0], func=AF.Copy)
        UK = ["uT0", "uT1"]
        yacc = B.alloc(regB, 2 * T)
        yacc3 = v3(yacc, 2)
        V("memset", [], ["yacc"], ap=yacc, constant=0.0)
        Cz = [B.alloc(regB, 8 * 128, BF16) for _ in range(2)]
        for ri in range(2):
            V("memset", [], [f"Cz{ri}"], ap=Cz[ri], constant=0.0)
            src = v3(pv(l, "cre" if ri == 0 else "cim", 0, 128), 8)
            for m in range(4):
                for hf in range(2):
                    dst = v3(Cz[ri], 8)[hf * 64:(hf + 1) * 64, m::4, 32 * m + 16 * hf: 32 * m + 16 * hf + 16]
                    V("tensor_scalar", ["pvec"], [f"Cz{ri}"], out=dst, in0=src[hf * 64:(hf + 1) * 64, m::4, :],
                      scalar1=(1.0 if ri == 0 else -1.0), scalar2=None, op0=ALU.mult)
        sm = B.alloc(regB, 64 * 8)

        def smt(i):
            return sm[:, i * 8:(i + 1) * 8]

        PWr = B.alloc(regB, (SQ + 1) * 8)
        PWi = B.alloc(regB, (SQ + 1) * 8)
        NPi = B.alloc(regB, (SQ + 1) * 8)
        MUr = B.alloc(regB, NLEV * 8)
        MUi = B.alloc(regB, NLEV * 8)
        NMi = B.alloc(regB, NLEV * 8)
        Bb = [B.alloc(regB, 128) for _ in range(2)]
        Zp = B.alloc(regB, 8 * 128, BF16)
        BzT = [B.alloc(regB, 8 * 128, BF16) for _ in range(2)]
        bus = [B.alloc(regA, 2 * T, BF16) for _ in range(2)]
        dgs = [B.alloc(regA, 3 * (SQ + 1) * 128, BF16) for _ in range(3)]
        xbfs = [B.alloc(regA, 2 * NSC, BF16) for _ in range(2)]
        itd = 0
        cnt5 = {"ith": 0}
        hbs = [B.alloc(regA, 2 * T, BF16) for _ in range(2)]
        xas = [B.alloc(regA, 2 * NSC) for _ in range(2)]
        xbs = [B.alloc(regA, 2 * NSC) for _ in range(2)]
        S = "s5s"
        for d in range(2):
            lre, lim, lst = pv(l, "lamre", d * 8, 8), pv(l, "lamim", d * 8, 8), pv(l, "lstep", d * 8, 8)
            step, th, lrs, mag, sn, cs_, cc, ss, ar, ai, den, am1, cr, ci, tmp, tmp2 = [smt(i) for i in range(16)]
            A(["pvec", S], [S], out=step, in_=lst, func=AF.Exp)
            V("tensor_tensor", [S, "pvec"], [S], out=th, in0=lim, in1=step, op=ALU.mult)
            V("tensor_tensor", [S, "pvec"], [S], out=lrs, in0=lre, in1=step, op=ALU.mult)
            A([S], [S], out=mag, in_=lrs, func=AF.Exp)
            A([S], [S], out=sn, in_=th, func=AF.Sin, scale=1.0 / 16.0)
            A([S], [S], out=cs_, in_=th, func=AF.Sin, scale=1.0 / 16.0, bias=math.pi / 2)
            for _ in range(4):
                V("tensor_tensor", [S], [S], out=cc, in0=cs_, in1=cs_, op=ALU.mult)
                V("tensor_tensor", [S], [S], out=ss, in0=sn, in1=sn, op=ALU.mult)
                V("scalar_tensor_tensor", [S], [S], out=sn, in0=cs_, scalar=2.0, in1=sn, op0=ALU.mult, op1=ALU.mult)
                V("tensor_tensor", [S], [S], out=cs_, in0=cc, in1=ss, op=ALU.subtract)
            V("tensor_tensor", [S], [S], out=ar, in0=mag, in1=cs_, op=ALU.mult)
            V("tensor_tensor", [S], [S], out=ai, in0=mag, in1=sn, op=ALU.mult)
            V("tensor_tensor", [S, "pvec"], [S], out=den, in0=lre, in1=lre, op=ALU.mult)
            V("tensor_tensor", [S, "pvec"], [S], out=tmp, in0=lim, in1=lim, op=ALU.mult)
            V("tensor_tensor", [S], [S], out=den, in0=den, in1=tmp, op=ALU.add)
            V("reciprocal", [S], [S], out=den, in_=den)
            V("tensor_scalar", [S], [S], out=am1, in0=ar, scalar1=-1.0, scalar2=None, op0=ALU.add)
            V("tensor_tensor", [S, "pvec"], [S], out=tmp, in0=am1, in1=lre, op=ALU.mult)
            V("tensor_tensor", [S, "pvec"], [S], out=tmp2, in0=ai, in1=lim, op=ALU.mult)
            V("tensor_tensor", [S], [S], out=cr, in0=tmp, in1=tmp2, op=ALU.add)
            V("tensor_tensor", [S], [S], out=cr, in0=cr, in1=den, op=ALU.mult)
            V("tensor_tensor", [S, "pvec"], [S], out=tmp, in0=ai, in1=lre, op=ALU.mult)
            V("tensor_tensor", [S, "pvec"], [S], out=tmp2, in0=am1, in1=lim, op=ALU.mult)
            V("tensor_tensor", [S], [S], out=ci, in0=tmp, in1=tmp2, op=ALU.subtract)
            V("tensor_tensor", [S], [S], out=ci, in0=ci, in1=den, op=ALU.mult)
            bre3, bim3 = v3(pv(l, "bre", 0, 128), 8), v3(pv(l, "bim", 0, 128), 8)
            crb = cr.unsqueeze(2).broadcast_to([128, 8, 16])
            cib = ci.unsqueeze(2).broadcast_to([128, 8, 16])
            t1 = B.alloc(regB, 128) if d == 0 else t1
            V("tensor_tensor", [S, "pvec"], ["Bb0"], out=v3(Bb[0], 8), in0=bre3, in1=crb, op=ALU.mult)
            V("tensor_tensor", [S, "pvec"], ["t1"], out=v3(t1, 8), in0=bim3, in1=cib, op=ALU.mult)
            V("tensor_tensor", ["Bb0", "t1"], ["Bb0"], out=Bb[0], in0=Bb[0], in1=t1, op=ALU.subtract)
            V("tensor_tensor", [S, "pvec"], ["Bb1"], out=v3(Bb[1], 8), in0=bim3, in1=crb, op=ALU.mult)
            V("tensor_tensor", [S, "pvec", "Bb0"], ["t1"], out=v3(t1, 8), in0=bre3, in1=cib, op=ALU.mult)
            V("tensor_tensor", ["Bb1", "t1"], ["Bb1"], out=Bb[1], in0=Bb[1], in1=t1, op=ALU.add)
            for ri in range(2):
                V("memset", [], ["Zp"], ap=Zp, constant=0.0)
                for m in range(4):
                    for hf in range(2):
                        dst = v3(Zp, 8)[hf * 64:(hf + 1) * 64, m::4, 32 * m + 16 * hf: 32 * m + 16 * hf + 16]
                        V("tensor_copy", [f"Bb{ri}"], ["Zp"], out=dst, in_=v3(Bb[ri], 8)[hf * 64:(hf + 1) * 64, m::4, :])
                psb = ps[5][:, 0:512].bitcast(BF16)
                TR([dict(out=psb[:, tt * 128:(tt + 1) * 128], in_=Zp[:, tt * 128:(tt + 1) * 128], ident=ident_b)
                    for tt in range(8)], ["Zp", "cb"], ["ps5"])
                V("tensor_copy", ["ps5"], [f"BzT{ri}"], out=BzT[ri], in_=psb)
            V("memset", [], [S], ap=PWr[:, 0:8], constant=1.0)
            V("memset", [], [S], ap=PWi[:, 0:8], constant=0.0)
            for j in range(1, SQ + 1):
                pr0, pi0 = PWr[:, (j - 1) * 8:j * 8], PWi[:, (j - 1) * 8:j * 8]
                pr1, pi1 = PWr[:, j * 8:(j + 1) * 8], PWi[:, j * 8:(j + 1) * 8]
                V("tensor_tensor", [S], [S], out=tmp, in0=pi0, in1=ai, op=ALU.mult)
                V("tensor_tensor", [S], [S], out=pr1, in0=pr0, in1=ar, op=ALU.mult)
                V("tensor_tensor", [S], [S], out=pr1, in0=pr1, in1=tmp, op=ALU.subtract)
                V("tensor_tensor", [S], [S], out=tmp, in0=pi0, in1=ar, op=ALU.mult)
                V("tensor_tensor", [S], [S], out=pi1, in0=pr0, in1=ai, op=ALU.mult)
                V("tensor_tensor", [S], [S], out=pi1, in0=pi1, in1=tmp, op=ALU.add)
            V("tensor_scalar", [S], [S], out=NPi, in0=PWi, scalar1=-1.0, scalar2=None, op0=ALU.mult)
            V("tensor_copy", [S], [S], out=MUr[:, 0:8], in_=PWr[:, SQ * 8:(SQ + 1) * 8])
            V("tensor_copy", [S], [S], out=MUi[:, 0:8], in_=PWi[:, SQ * 8:(SQ + 1) * 8])
            for k in range(1, NLEV):
                mr0, mi0 = MUr[:, (k - 1) * 8:k * 8], MUi[:, (k - 1) * 8:k * 8]
                mr1, mi1 = MUr[:, k * 8:(k + 1) * 8], MUi[:, k * 8:(k + 1) * 8]
                V("tensor_tensor", [S], [S], out=tmp, in0=mi0, in1=mi0, op=ALU.mult)
                V("tensor_tensor", [S], [S], out=mr1, in0=mr0, in1=mr0, op=ALU.mult)
                V("tensor_tensor", [S], [S], out=mr1, in0=mr1, in1=tmp, op=ALU.subtract)
                V("scalar_tensor_tensor", [S], [S], out=mi1, in0=mr0, scalar=2.0, in1=mi0, op0=ALU.mult, op1=ALU.mult)
            V("tensor_scalar", [S], [S], out=NMi, in0=MUi, scalar1=-1.0, scalar2=None, op0=ALU.mult)

            def cmap(n0, n1):
                if d == 0:
                    return n0, n1
                if n0 < TC:
                    return 2048 + n0, 2048 + n1
                return n0 - TC, n1 - TC

            def build_diag(tt_):
                dg_ = dgs[tt_ % 3]
                for j in range(SQ + 1):
                    for v_, tab in enumerate((PWr, PWi, NPi)):
                        V("tensor_scalar", ["consts", S], [f"dg{tt_ % 3}"], out=dg_[:, (j * 3 + v_) * 128:(j * 3 + v_ + 1) * 128], in0=ident_f,
                          scalar1=tab[:, j * 8 + tt_: j * 8 + tt_ + 1], scalar2=None, op0=ALU.mult)

            def s5_iter(tt, dgp):
                hfc = tt // 4
                bu4, hb3 = v4(bus[dgp], 2, SQ), v3(hbs[dgp], 2)
                xa, xb_, xbf = xas[dgp], xbs[dgp], xbfs[dgp]
                BU, XK_, XBF, HB = f"bu{dgp}", f"x{dgp}", f"xbf{dgp}", f"hb{dgp}"
                for ti, (n0, n1) in enumerate(NTILES):
                    b0, b1 = cmap(n0, n1)
                    for ri in range(2):
                        pi = 4 + ((ti * 2 + ri) % 2)
                        MM([dict(out=ps[pi][:, 0:n1 - n0], lhsT=v3(BzT[ri], 8)[:, tt, :], rhs=uT3[:, hfc, n0:n1])],
                           [f"BzT{ri}", f"uT{hfc}"], [f"ps{pi}"])
                        A([f"ps{pi}"], [BU], out=bu4[:, ri, :, b0 // SQ:b1 // SQ],
                          in_=ps[pi][:, 0:n1 - n0].rearrange("p (c t) -> p t c", t=SQ), func=AF.Copy)

                def sc(tab, j):
                    return tab[:, j * 8 + tt: j * 8 + tt + 1]

                dg = dgs[tt % 3]
                dk = f"dg{tt % 3}"

                def dgm(j, v_):
                    return dg[:, (j * 3 + v_) * 128:(j * 3 + v_ + 1) * 128]

                def cterms(j, src_r, src_i, out_r, out_i, first, last):
                    mr = [dict(out=out_r, lhsT=dgm(j, 0), rhs=src_r, start=first, stop=False),
                          dict(out=out_r, lhsT=dgm(j, 2), rhs=src_i, start=False, stop=last)]
                    mi = [dict(out=out_i, lhsT=dgm(j, 1), rhs=src_r, start=first, stop=False),
                          dict(out=out_i, lhsT=dgm(j, 0), rhs=src_i, start=False, stop=last)]
                    return mr, mi

                mr_all, mi_all = [], []
                for s_ in range(SQ):
                    j = SQ - 1 - s_ if d == 0 else s_
                    mr, mi = cterms(j, bu4[:, 0, s_, :], bu4[:, 1, s_, :], ps[6][:, 0:NSC], ps[7][:, 0:NSC], s_ == 0, s_ == SQ - 1)
                    mr_all += mr
                    mi_all += mi
                MM(mr_all, [BU, dk], ["ps6"])
                MM(mi_all, [BU, dk], ["ps7"])
                xa3, xb3 = v3(xa, 2), v3(xb_, 2)
                A(["ps6"], [XK_], out=xa3[:, 0, :], in_=ps[6][:, 0:NSC], func=AF.Copy)
                A(["ps7"], [XK_], out=xa3[:, 1, :], in_=ps[7][:, 0:NSC], func=AF.Copy)
                if tt + 1 < 8:
                    build_diag(tt + 1)
                cur, nxt = xa3, xb3
                for k in range(NLEV):
                    s_ = 1 << k
                    V("tensor_copy", [XK_], [XK_], out=nxt, in_=cur)
                    if d == 0:
                        dr, di, sr, si = nxt[:, 0, s_:], nxt[:, 1, s_:], cur[:, 0, 0:NSC - s_], cur[:, 1, 0:NSC - s_]
                    else:
                        dr, di, sr, si = nxt[:, 0, 0:NSC - s_], nxt[:, 1, 0:NSC - s_], cur[:, 0, s_:], cur[:, 1, s_:]
                    for (o_, i_, scl) in ((dr, sr, sc(MUr, k)), (dr, si, sc(NMi, k)), (di, si, sc(MUr, k)), (di, sr, sc(MUi, k))):
                        V("scalar_tensor_tensor", [XK_, S], [XK_], out=o_, in0=i_, scalar=scl, in1=o_, op0=ALU.mult, op1=ALU.add)
                    cur, nxt = nxt, cur
                V("tensor_copy", [XK_], [XBF], out=xbf, in_=cur)
                xbf3 = v3(xbf, 2)

                def stage2():
                    for tau in range(SQ):
                        hp = cnt5["ith"] % 2
                        cnt5["ith"] += 1
                        o_r, o_i = ps[0 + hp][:, 0:NSC], ps[2 + hp][:, 0:NSC]
                        srcs = list(range(0, tau + 1)) if d == 0 else list(range(tau, SQ))
                        mr_all, mi_all = [], []
                        for n_, s_ in enumerate(srcs):
                            mr, mi = cterms(abs(tau - s_), bu4[:, 0, s_, :], bu4[:, 1, s_, :], o_r, o_i, n_ == 0, False)
                            mr_all += mr
                            mi_all += mi
                        if d == 0:
                            mr, mi = cterms(tau + 1, xbf3[:, 0, 0:NSC - 1], xbf3[:, 1, 0:NSC - 1], ps[0 + hp][:, 1:NSC], ps[2 + hp][:, 1:NSC], False, True)
                        else:
                            mr, mi = cterms(SQ - tau, xbf3[:, 0, 1:NSC], xbf3[:, 1, 1:NSC], ps[0 + hp][:, 0:NSC - 1], ps[2 + hp][:, 0:NSC - 1], False, True)
                        mr_all += mr
                        mi_all += mi
                        MM(mr_all, [BU, dk, XBF], [f"ps{0 + hp}"])
                        MM(mi_all, [BU, dk, XBF], [f"ps{2 + hp}"])
                        A([f"ps{0 + hp}"], [HB], out=hb3[:, 0, tau::SQ], in_=o_r, func=AF.Copy)
                        A([f"ps{2 + hp}"], [HB], out=hb3[:, 1, tau::SQ], in_=o_i, func=AF.Copy)
                    for ti, (n0, n1) in enumerate(NTILES):
                        b0, b1 = cmap(n0, n1)
                        pi = ti % 4
                        MM([dict(out=ps[pi][:, 0:n1 - n0], lhsT=v3(Cz[ri], 8)[:, tt, :], rhs=hb3[:, ri, b0:b1],
                                 start=(ri == 0), stop=(ri == 1)) for ri in range(2)], ["Cz0", "Cz1", HB], [f"ps{pi}"])
                        V("tensor_tensor", [f"ps{pi}", "yacc"], ["yacc"], out=yacc3[:, hfc, n0:n1], in0=yacc3[:, hfc, n0:n1],
                          in1=ps[pi][:, 0:n1 - n0], op=ALU.add)
                return stage2

            pend2 = None
            build_diag(0)
            for tt in range(8):
                dgp = itd % 2
                itd += 1
                nxt2 = s5_iter(tt, dgp)
                if pend2 is not None:
                    pend2()
                pend2 = nxt2
            pend2()
        wg = B.alloc(regB, 2 * 512, BF16)
        t.dma("pool", wg, wglu_d[l], w=["wglu"], sem="d_wglu")
        wg3 = v3(wg, 2)
        yg = B.alloc(regB, 2 * T, BF16)
        yg3 = v3(yg, 2)
        for c in range(2):
            V("scalar_tensor_tensor", [f"uT{c}", "yacc", "pvec"], ["yacc"], out=yacc3[:, c, :], in0=uT3[:, c, :],
              scalar=pv(l, "s5_d", c), in1=yacc3[:, c, :], op0=ALU.mult, op1=ALU.add)
            A(["yacc"], [f"yg{c}"], out=yg3[:, c, :], in_=yacc3[:, c, :], func=AF.Gelu_apprx_tanh)
        sg = [B.alloc(regB, 512) for _ in range(2)]
        for c in range(2):
            for ti, (n0, n1) in enumerate(NT_l):
                n = n1 - n0
                pa, pb = 3 + (ti % 2), 6 + (ti % 2)
                MM([dict(out=ps[pa][:, 0:n], lhsT=wg3[:, kt, c * 128:(c + 1) * 128], rhs=yg3[:, kt, n0:n1],
                         start=(kt == 0), stop=(kt == 1)) for kt in range(2)], ["wglu", "yg0", "yg1"], [f"ps{pa}"])
                MM([dict(out=ps[pb][:, 0:n], lhsT=wg3[:, kt, 256 + c * 128:256 + (c + 1) * 128], rhs=yg3[:, kt, n0:n1],
                         start=(kt == 0), stop=(kt == 1)) for kt in range(2)], ["wglu", "yg0", "yg1"], [f"ps{pb}"])
                A([f"ps{pb}", "pvec"], [f"sg{ti % 2}"], out=sg[ti % 2][:, 0:n], in_=ps[pb][:, 0:n], func=AF.Sigmoid,
                  bias=pv(l, "glu_b", 2 + c), scale=1.0)
                V("scalar_tensor_tensor", [f"ps{pa}", f"sg{ti % 2}", "pvec"], [f"mixbs{2 + c}"], out=mix3[:, 2 + c, n0:n1],
                  in0=ps[pa][:, 0:n], scalar=pv(l, "glu_b", c), in1=sg[ti % 2][:, 0:n], op0=ALU.add, op1=ALU.mult)
        if "s5" in B.dbg and l == 0:
            t.barrier()
            reset(regA)
            B.regD = regA
            B.dump_any("s5", mixbs[:, 2 * T:4 * T], 2 * T, ["mixbs2", "mixbs3"], BF16)
        t.barrier()
        if stop == "s5":
            break


        reset(regA)
        reset(regB)
        wslots_in = [B.alloc(regB, 8 * 128, BF16) for _ in range(3)]
        USW = 2364
        ustg = B.alloc(regA, 2 * USW, BF16)
        ustg3 = v3(ustg, 2)
        V("memset", [], ["ustg"], ap=ustg, constant=0.0)
        diagC = B.alloc(regA, 62 * 128, BF16)
        for c in range(2):
            for k in range(31):
                V("tensor_scalar", ["consts", "pvec"], ["diagC"], out=diagC[:, (c * 31 + k) * 128:(c * 31 + k + 1) * 128],
                  in0=ident_f, scalar1=pv(l, "dw_w", k * 2 + c), scalar2=None, op0=ALU.mult)
        wpw = B.alloc(regA, 512, BF16)
        t.dma("pool", wpw, wpw_d[l], w=["wpw"], sem="d_wpw")
        wpw3 = v3(wpw, 2)
        cv = B.alloc(regA, 2 * T)
        cv3 = v3(cv, 2)
        csb = B.alloc(regA, 2 * T, BF16)
        csb3 = v3(csb, 2)
        sig = [B.alloc(regB, 512) for _ in range(2)]
        sqf = [B.alloc(regB, 512) for _ in range(2)]
        mt_, m2_, var_, xn_ = [B.alloc(regB, 512) for _ in range(4)]
        it = 0
        for c in range(2):
            wtv, wkv = load_win_tile(l, J_CONF + c, wslots_in)
            wtg, wkg = load_win_tile(l, J_CONF + 2 + c, wslots_in)
            for ti, (n0, n1) in enumerate(NT_l):
                n = n1 - n0
                pa, pb = 3 + (it % 2), 5 + (it % 2)
                sgi = it % 2
                it += 1
                proj(wtv, wkv, n0, n1, ps[pa], f"ps{pa}")
                proj(wtg, wkg, n0, n1, ps[pb], f"ps{pb}")
                A([f"ps{pb}"], [f"sig{sgi}"], out=sig[sgi][:, 0:n], in_=ps[pb][:, 0:n], func=AF.Sigmoid)
                po = 15 + n0 if n0 < TC else n0 + 45
                V("tensor_tensor", [f"ps{pa}", f"sig{sgi}"], ["ustg"], out=ustg3[:, c, po:po + n], in0=ps[pa][:, 0:n],
                  in1=sig[sgi][:, 0:n], op=ALU.mult)
        it = 0
        for c in range(2):
            for ti, (n0, n1) in enumerate(NT_l):
                n = n1 - n0
                o0 = n0 if n0 < TC else n0 + 30
                pa = 3 + (it % 2)
                it += 1
                MM([dict(out=ps[pa][:, 0:n], lhsT=diagC[:, (c * 31 + k) * 128:(c * 31 + k + 1) * 128],
                         rhs=ustg3[:, c, o0 + k:o0 + k + n], start=(k == 0), stop=(k == 30)) for k in range(31)],
                   ["diagC", "ustg"], [f"ps{pa}"])
                A([f"ps{pa}", "pvec"], [f"cv{c}"], out=cv3[:, c, n0:n1], in_=ps[pa][:, 0:n], func=AF.Identity,
                  bias=pv(l, "dw_b", c), scale=1.0)
        for ti, (n0, n1) in enumerate(NT_l):
            n = n1 - n0
            for c in range(2):
                A([f"cv{c}"], [f"sqf{c}"], out=sqf[c][:, 0:n], in_=cv3[:, c, n0:n1], func=AF.Square)
            MM([dict(out=ps[1][:, 0:n], lhsT=ones_f, rhs=cv3[:, c, n0:n1], start=(c == 0), stop=(c == 1)) for c in range(2)],
               ["cv0", "cv1", "consts"], ["ps1"])
            MM([dict(out=ps[2][:, 0:n], lhsT=ones_f, rhs=sqf[c][:, 0:n], start=(c == 0), stop=(c == 1)) for c in range(2)],
               ["sqf0", "sqf1", "consts"], ["ps2"])
            V("tensor_scalar", ["ps1"], ["lnm"], out=mt_[:, 0:n], in0=ps[1][:, 0:n], scalar1=1.0 / 256.0, scalar2=None, op0=ALU.mult)
            V("tensor_tensor", ["lnm"], ["lnm2"], out=m2_[:, 0:n], in0=mt_[:, 0:n], in1=mt_[:, 0:n], op=ALU.mult)
            V("scalar_tensor_tensor", ["ps2", "lnm2"], ["lnv"], out=var_[:, 0:n], in0=ps[2][:, 0:n], scalar=1.0 / 256.0,
              in1=m2_[:, 0:n], op0=ALU.mult, op1=ALU.subtract)
            A(["lnv"], ["lnv"], out=var_[:, 0:n], in_=var_[:, 0:n], func=AF.Sqrt, bias=EPS, scale=1.0)
            V("reciprocal", ["lnv"], ["lnv"], out=var_[:, 0:n], in_=var_[:, 0:n])
            for c in range(2):
                V("tensor_tensor", [f"cv{c}", "lnm"], ["lnx"], out=xn_[:, 0:n], in0=cv3[:, c, n0:n1], in1=mt_[:, 0:n], op=ALU.subtract)
                V("tensor_tensor", ["lnx", "lnv"], ["lnx"], out=xn_[:, 0:n], in0=xn_[:, 0:n], in1=var_[:, 0:n], op=ALU.mult)
                A(["lnx", "pvec"], [f"csb{c}"], out=csb3[:, c, n0:n1], in_=xn_[:, 0:n], func=AF.Silu,
                  scale=pv(l, "ln_g", c), bias=pv(l, "ln_b", c))
            for c2 in range(2):
                pa = 3 + c2
                MM([dict(out=ps[pa][:, 0:n], lhsT=wpw3[:, c, c2 * 128:(c2 + 1) * 128], rhs=csb3[:, c, n0:n1],
                         start=(c == 0), stop=(c == 1)) for c in range(2)], ["wpw", "csb0", "csb1"], [f"ps{pa}"])
                A([f"ps{pa}", "pvec"], [f"mixbs{c2}"], out=mix3[:, c2, n0:n1], in_=ps[pa][:, 0:n], func=AF.Identity,
                  bias=pv(l, "pw_b", c2), scale=1.0)
        if "conf" in B.dbg and l == 0:
            t.barrier()
            reset(regA)
            B.regD = regA
            B.dump_any("conf", mixbs[:, 0:2 * T], 2 * T, ["mixbs0", "mixbs1"], BF16)
        t.barrier()
        if stop == "conf":
            break

        reset(regA)
        reset(regB)
        wslots_in = [B.alloc(regB, 8 * 128, BF16) for _ in range(3)]
        zS = B.alloc(regA, 4 * T, BF16)
        zS3 = v3(zS, 4)
        xbcT = B.alloc(regA, 8 * T, BF16)
        xbc3 = v3(xbcT, 8)
        PRW = 2312
        pre = [B.alloc(regA, PRW, BF16) for _ in range(2)]
        diagS = B.alloc(regB, 40 * 128, BF16)
        for j in range(8):
            for k in range(5):
                V("tensor_scalar", ["consts", "pvec"], ["diagS"], out=diagS[:, (j * 5 + k) * 128:(j * 5 + k + 1) * 128],
                  in0=ident_f, scalar1=pv(l, "conv_w", k * 8 + j), scalar2=None, op0=ALU.mult)
        for i in range(2):
            V("memset", [], [f"pre{i}"], ap=pre[i], constant=0.0)
        it = 0
        for j in range(4):
            wt, wk = load_win_tile(l, J_Z + j, wslots_in)
            for ti, (n0, n1) in enumerate(NT_l):
                pa = 3 + (it % 2)
                it += 1
                proj(wt, wk, n0, n1, ps[pa], f"ps{pa}")
                A([f"ps{pa}"], [f"zS{j}"], out=zS3[:, j, n0:n1], in_=ps[pa][:, 0:n1 - n0], func=AF.Silu)
        for j in range(8):
            wt, wk = load_win_tile(l, J_XBC + j, wslots_in)
            pr = pre[j % 2]
            pk = f"pre{j % 2}"
            for ti, (n0, n1) in enumerate(NTILES):
                pa = 3 + (it % 2)
                it += 1
                proj(wt, wk, n0, n1, ps[pa], f"ps{pa}")
                po = 2 + n0 if n0 < TC else n0 + 6
                A([f"ps{pa}"], [pk], out=pr[:, po:po + n1 - n0], in_=ps[pa][:, 0:n1 - n0], func=AF.Copy)
            for ti, (n0, n1) in enumerate(NTILES):
                n = n1 - n0
                o0 = n0 if n0 < TC else n0 + 4
                pa = 5 + (it % 2)
                it += 1
                MM([dict(out=ps[pa][:, 0:n], lhsT=diagS[:, (j * 5 + k) * 128:(j * 5 + k + 1) * 128], rhs=pr[:, o0 + k:o0 + k + n],
                         start=(k == 0), stop=(k == 4)) for k in range(5)], ["diagS", pk], [f"ps{pa}"])
                A([f"ps{pa}", "pvec"], [f"xbc{j}"], out=xbc3[:, j, n0:n1], in_=ps[pa][:, 0:n], func=AF.Silu,
                  bias=pv(l, "conv_b", j), scale=1.0)
        wt, wk = load_win_tile(l, J_DT, wslots_in)
        MM([dict(out=ps[1][:, c * 16:(c + 1) * 16], lhsT=hT3[:, kc, c * 128:(c + 1) * 128], rhs=wt[:, kc, 0:16],
                 start=(kc == 0), stop=(kc == 7)) for c in range(NCH) for kc in range(8)], [wk] + HK, ["ps1"])
        NX = NCH * 16
        xdt, e_, dtv, Atok, EcsmA, ErcsmA, Etot, dteF, dteB, tm_ = [B.alloc(regB, NX) for _ in range(10)]
        aneg = B.alloc(regB, 16)
        dtb = pbc[:, l * NPB:l * NPB + 16]
        alog = pbc[:, l * NPB + 16:l * NPB + 32]
        D_ = "dts"
        V("tensor_tensor", ["ps1", "pbc"], [D_], out=v3(xdt, NCH), in0=v3(ps[1][:, 0:NX], NCH),
          in1=dtb.unsqueeze(1).broadcast_to([128, NCH, 16]), op=ALU.add)
        V("tensor_scalar", [D_], [D_], out=e_, in0=xdt, scalar1=30.0, scalar2=None, op0=ALU.min)
        A([D_], [D_], out=e_, in_=e_, func=AF.Exp)
        A([D_], [D_], out=e_, in_=e_, func=AF.Ln, bias=1.0, scale=1.0)
        V("tensor_tensor", [D_], [D_], out=dtv, in0=e_, in1=xdt, op=ALU.max)
        A(["pbc", D_], [D_], out=aneg, in_=alog, func=AF.Exp)
        V("tensor_scalar", [D_], [D_], out=aneg, in0=aneg, scalar1=-1.0, scalar2=None, op0=ALU.mult)
        V("tensor_tensor", [D_], [D_], out=v3(Atok, NCH), in0=v3(dtv, NCH), in1=aneg.unsqueeze(1).broadcast_to([128, NCH, 16]), op=ALU.mult)
        MM([dict(out=ps[1][:, 0:NX], lhsT=LE_f, rhs=Atok)], [D_, "consts"], ["ps1"])
        MM([dict(out=ps[2][:, 0:NX], lhsT=GE_f, rhs=Atok)], [D_, "consts"], ["ps2"])
        MM([dict(out=ps[3][:, 0:NX], lhsT=ones_f, rhs=Atok)], [D_, "consts"], ["ps3"])
        A(["ps3"], [D_], out=Etot, in_=ps[3][:, 0:NX], func=AF.Exp)
        V("tensor_tensor", ["ps1", D_], [D_], out=tm_, in0=ps[1][:, 0:NX], in1=Atok, op=ALU.subtract)
        A([D_], [D_], out=EcsmA, in_=tm_, func=AF.Exp)
        V("tensor_tensor", ["ps2", D_], [D_], out=tm_, in0=ps[2][:, 0:NX], in1=Atok, op=ALU.subtract)
        A([D_], [D_], out=ErcsmA, in_=tm_, func=AF.Exp)
        V("tensor_tensor", [D_], [D_], out=v3(dteF, NCH)[:, :, 0:8], in0=v3(dtv, NCH)[:, :, 0:8], in1=v3(ErcsmA, NCH)[:, :, 0:8], op=ALU.mult)
        V("tensor_tensor", [D_], [D_], out=v3(dteB, NCH)[:, :, 0:8], in0=v3(dtv, NCH)[:, :, 8:16], in1=v3(EcsmA, NCH)[:, :, 8:16], op=ALU.mult)
        mark = regB[1]
        tok2 = [B.alloc(regB, 768, BF16) for _ in range(2)]
        Xb = [B.alloc(regB, 512, BF16) for _ in range(2)]
        Xdb = [B.alloc(regB, 512, BF16) for _ in range(2)]
        Wm = B.alloc(regB, 1024)
        expD = [B.alloc(regB, 1024, BF16) for _ in range(2)]
        einb = [B.alloc(regB, 1024, BF16) for _ in range(2)]
        Gm = [B.alloc(regB, 256, BF16) for _ in range(2)]
        MTb = [B.alloc(regB, 1024, BF16) for _ in range(2)]
        Ceb = [B.alloc(regB, 1024, BF16) for _ in range(2)]
        ytmp = B.alloc(regB, 512)
        S32 = B.alloc(regB, 512)
        Sb = B.alloc(regB, 512, BF16)
        it = 0
        S32s = [S32, B.alloc(regB, 512)]
        Sbs = [Sb, B.alloc(regB, 512, BF16)]
        for d in range(2):
            V("memset", [], [f"S32{d}"], ap=S32s[d], constant=0.0)
            V("memset", [], [f"Sb{d}"], ap=Sbs[d], constant=0.0)
        for j in range(4):
            V("tensor_scalar", [f"xbc{j}", "pvec"] + HK, [f"hT{j}"], out=hT3[:, j, :], in0=xbc3[:, j, :], scalar1=pv(l, "ssd_d", j),
              scalar2=None, op0=ALU.mult)
        orders = [list(range(NCH)), [1, 0] + list(range(NCH - 1, 1, -1))]
        def ssd_iter(step, d, p_):
            hoff = 8 * d
            maskW = LE_f if d == 0 else GE_f
            Dl = GT_f if d == 0 else LT_f
            dte = dteF if d == 0 else dteB
            S32, Sb = S32s[d], Sbs[d]
            SK32, SKb = f"S32{d}", f"Sb{d}"
            c = orders[d][step]
            c0, c1 = c * 128, (c + 1) * 128
            V("tensor_tensor", ["consts", D_], ["Wm"], out=v3(Wm, 8), in0=maskW.unsqueeze(1).broadcast_to([128, 8, 128]),
              in1=v3(Atok, NCH)[:, c, hoff:hoff + 8].unsqueeze(2).broadcast_to([128, 8, 128]), op=ALU.mult)
            psb = ps[3][:, 0:384].bitcast(BF16)
            TR([dict(out=psb[:, i * 128:(i + 1) * 128], in_=xbc3[:, i, c0:c1], ident=ident_b) for i in range(6)],
               [f"xbc{i}" for i in range(6)] + ["cb"], ["ps3"])
            A(["ps3"], [f"tok{p_}"], out=tok2[p_], in_=psb, func=AF.Copy)
            xs_tok, B_tok = tok2[p_][:, 0:512], tok2[p_][:, 512:768]
            V("tensor_tensor", [f"tok{p_}", D_], [f"X{p_}"], out=v3(Xb[p_], 8), in0=v3(xs_tok, 8),
              in1=v3(dtv, NCH)[:, c, hoff:hoff + 8].unsqueeze(2).broadcast_to([128, 8, 64]), op=ALU.mult)
            V("tensor_tensor", [f"tok{p_}", D_], [f"Xd{p_}"], out=v3(Xdb[p_], 8), in0=v3(xs_tok, 8),
              in1=v3(dte, NCH)[:, c, 0:8].unsqueeze(2).broadcast_to([128, 8, 64]), op=ALU.mult)
            MM([dict(out=ps[4][:, 0:512], lhsT=Dl, rhs=Wm[:, 0:512]), dict(out=ps[5][:, 0:512], lhsT=Dl, rhs=Wm[:, 512:1024])],
               ["Wm", "consts"], ["ps4", "ps5"])
            MM([dict(out=ps[6][:, 0:512], lhsT=ones_f, rhs=Wm[:, 0:512]), dict(out=ps[7][:, 0:512], lhsT=ones_f, rhs=Wm[:, 512:1024])],
               ["Wm", "consts"], ["ps6", "ps7"])
            A(["ps4"], [f"expD{p_}a"], out=expD[p_][:, 0:512], in_=ps[4][:, 0:512], func=AF.Exp)
            A(["ps5"], [f"expD{p_}b"], out=expD[p_][:, 512:1024], in_=ps[5][:, 0:512], func=AF.Exp)
            A(["ps6"], [f"ein{p_}a"], out=einb[p_][:, 0:512], in_=ps[6][:, 0:512], func=AF.Exp)
            A(["ps7"], [f"ein{p_}b"], out=einb[p_][:, 512:1024], in_=ps[7][:, 0:512], func=AF.Exp)
            MM([dict(out=ps[0][:, g * 128:(g + 1) * 128], lhsT=xbc3[:, 4 + g, c0:c1], rhs=xbc3[:, 6 + g, c0:c1]) for g in range(2)],
               ["xbc4", "xbc5", "xbc6", "xbc7"], ["ps0"])
            V("tensor_tensor", ["ps0", "consts"], [f"Gm{p_}"], out=v3(Gm[p_], 2), in0=v3(ps[0][:, 0:256], 2),
              in1=maskW.unsqueeze(1).broadcast_to([128, 2, 128]), op=ALU.mult)
            V("tensor_tensor", [f"expD{p_}a", f"expD{p_}b", f"Gm{p_}"], [f"MT{p_}"], out=v4(MTb[p_], 2, 4), in0=v4(expD[p_], 2, 4),
              in1=v3(Gm[p_], 2).unsqueeze(2).broadcast_to([128, 2, 4, 128]), op=ALU.mult)
            V("tensor_tensor", [f"ein{p_}a", f"ein{p_}b", "xbc6", "xbc7"], [f"Ce{p_}"], out=v4(Ceb[p_], 2, 4), in0=v4(einb[p_], 2, 4),
              in1=xbc3[:, 6:8, c0:c1].unsqueeze(2).broadcast_to([128, 2, 4, 128]), op=ALU.mult)

            def stageB():
                mms = []
                for h in range(8):
                    o_ = ps[1][(h % 2) * 64:(h % 2) * 64 + 64, (h // 2) * 128:(h // 2 + 1) * 128]
                    mms.append(dict(out=o_, lhsT=Xb[p_][:, h * 64:(h + 1) * 64], rhs=MTb[p_][:, h * 128:(h + 1) * 128], start=True, stop=False))
                    mms.append(dict(out=o_, lhsT=Sb[:, h * 64:(h + 1) * 64], rhs=Ceb[p_][:, h * 128:(h + 1) * 128], start=False, stop=True))
                MM(mms, [f"X{p_}", f"MT{p_}", SKb, f"Ce{p_}"], ["ps1"])
                yk = [f"hT{j}" for j in range(4)]
                V("tensor_tensor", yk + ["ps1"], yk, out=hT3[:, 0:4, c0:c1], in0=hT3[:, 0:4, c0:c1], in1=v3(ps[1][:, 0:512], 4), op=ALU.add)
                MM([dict(out=ps[2][:, g * 256:(g + 1) * 256], lhsT=B_tok[:, g * 128:(g + 1) * 128], rhs=Xdb[p_][:, g * 256:(g + 1) * 256])
                    for g in range(2)], [f"tok{p_}", f"Xd{p_}"], ["ps2"])
                V("tensor_tensor", [SK32, D_], [SK32], out=v3(S32, 8), in0=v3(S32, 8),
                  in1=v3(Etot, NCH)[:, c, hoff:hoff + 8].unsqueeze(2).broadcast_to([128, 8, 64]), op=ALU.mult)
                V("tensor_tensor", [SK32, "ps2"], [SK32], out=S32, in0=S32, in1=ps[2][:, 0:512], op=ALU.add)
                V("tensor_copy", [SK32], [SKb], out=Sb, in_=S32)
            return stageB

        pendB = None
        for step in range(NCH):
            for d in range(2):
                nxtB = ssd_iter(step, d, it % 2)
                it += 1
                if pendB is not None:
                    pendB()
                pendB = nxtB
        pendB()
        t.barrier()
        regB[1] = mark
        y2 = B.alloc(regB, 4 * 512)
        sq4 = B.alloc(regB, 4 * 512, BF16)
        rs_ = B.alloc(regB, 512)
        for ti, (n0, n1) in enumerate(NT_l):
            n = n1 - n0
            V("tensor_tensor", [f"hT{j}" for j in range(4)] + [f"zS{j}" for j in range(4)], ["y2"], out=v3(y2, 4)[:, :, 0:n],
              in0=hT3[:, 0:4, n0:n1], in1=zS3[:, :, n0:n1], op=ALU.mult)
            A(["y2"], ["sq4"], out=v3(sq4, 4)[:, :, 0:n], in_=v3(y2, 4)[:, :, 0:n], func=AF.Square)
            MM([dict(out=ps[1][:, 0:n], lhsT=ones_b, rhs=v3(sq4, 4)[:, j, 0:n], start=(j == 0), stop=(j == 3)) for j in range(4)],
               ["sq4", "cb"], ["ps1"])
            A(["ps1"], ["rs_"], out=rs_[:, 0:n], in_=ps[1][:, 0:n], func=AF.Sqrt, bias=EPS, scale=1.0 / 512.0)
            V("reciprocal", ["rs_"], ["rs_"], out=rs_[:, 0:n], in_=rs_[:, 0:n])
            for j in range(4):
                V("scalar_tensor_tensor", ["y2", "rs_", "pvec"], [f"hT{j}"], out=hT3[:, j, n0:n1], in0=v3(y2, 4)[:, j, 0:n],
                  scalar=pv(l, "ssd_ng", j), in1=rs_[:, 0:n], op0=ALU.mult, op1=ALU.mult)
        if "ssd" in B.dbg and l == 0:
            t.barrier()
            reset(regA)
            B.regD = regA
            B.dump_any("ssd", hT[:, 0:4 * T], 4 * T, [f"hT{j}" for j in range(4)], BF16)
        t.barrier()
        if stop == "ssd":
            break

        reset(regA)
        reset(regB)
        for k in range(8):
            t.dma("sp", x3[:, k, :], xsp[:, k * T:(k + 1) * T], r=[f"xsp{k}"], w=[f"xT{k}"])
        wo = B.alloc(regB, 8 * 1024, BF16)
        wo3 = v3(wo, 8)
        for i in range(2):
            t.dma("pool", wo[:, i * 4096:(i + 1) * 4096], wout_d[l][:, i * 4096:(i + 1) * 4096], w=[f"wo{i}"])
        it = 0
        for ti, (n0, n1) in enumerate(NT_l):
            n = n1 - n0
            col = 1 if n0 < TC else 0
            for dch in range(8):
                pa = 3 + (it % 4)
                it += 1
                mms = []
                for kt in range(8):
                    rhs = hT3[:, kt, n0:n1] if kt < 4 else mix3[:, kt - 4, n0:n1]
                    mms.append(dict(out=ps[pa][:, 0:n], lhsT=wo3[:, kt, dch * 128:(dch + 1) * 128], rhs=rhs, start=(kt == 0), stop=(kt == 7)))
                MM(mms, ["wo0", "wo1"] + [f"hT{j}" for j in range(4)] + [f"mixbs{j}" for j in range(4)], [f"ps{pa}"])
                V("scalar_tensor_tensor", [f"ps{pa}", "modT", f"xT{dch}"], [f"xT{dch}"], out=x3[:, dch, n0:n1], in0=ps[pa][:, 0:n],
                  scalar=modc(l, 16 + dch, col), in1=x3[:, dch, n0:n1], op0=ALU.mult, op1=ALU.add)
        if "xmid" in B.dbg and l == 0:
            B.dump("xmid", xT, 8 * T, XK)
        t.barrier()
        if stop == "xmid":
            break

        reset(regMB)
        tokssq = B.alloc(regMB, NCH)
        GTb = B.alloc(regMB, T, BF16)
        selE = B.alloc(regMB, 16 * 128, BF16)
        mark = regMB[1]
        rmsnorm_to_h(l, 1, regMB, tok_ssq=tokssq, ntiles=NT_l)
        NG = NCH * 16
        rwm = [B.alloc(regMB, 128) for _ in range(2)]
        shb = [B.alloc(regMB, 8 * 128) for _ in range(2)]
        lg, pm, tq, e1, e2 = [B.alloc(regMB, NG) for _ in range(5)]
        cbias = B.alloc(regMB, 32)
        rst = B.alloc(regMB, NCH)
        sm2 = B.alloc(regMB, 24 * NCH * 4)
        R_ = "rt"
        rb = pbc[:, l * NPB + 32:l * NPB + 48]
        der2 = v3(der[:, l * 32 + 16:l * 32 + 32], 8)
        for col in range(2):
            V("tensor_tensor", ["rwf", "der"], [R_], out=v3(rwm[col], 8), in0=v3(rwf, 8), in1=der2[:, :, col:col + 1].broadcast_to([128, 8, 16]), op=ALU.mult)
            for k in range(8):
                V("tensor_copy", ["modT"], [R_], out=shb[col][:, k * 128:(k + 1) * 128], in_=modc(l, 24 + k, col).broadcast_to([128, 128]))
            MM([dict(out=ps[3][:, col * 16:(col + 1) * 16], lhsT=shb[col][:, k * 128:(k + 1) * 128], rhs=v3(rwf, 8)[:, k, :],
                     start=(k == 0), stop=(k == 7)) for k in range(8)], [R_, "rwf"], ["ps3"])
        V("tensor_tensor", ["ps3", "pbc"], [R_], out=v3(cbias, 2), in0=v3(ps[3][:, 0:32], 2), in1=rb.unsqueeze(1).broadcast_to([128, 2, 16]), op=ALU.add)
        MM([dict(out=ps[4][:, c * 16:(c + 1) * 16], lhsT=x3[:, k, c * 128:(c + 1) * 128], rhs=v3(rwm[1 if c < 2 else 0], 8)[:, k, :],
                 start=(k == 0), stop=(k == 7)) for c in range(NCH) for k in range(8)], [R_] + XK, ["ps4"])
        A(["tokssq"], [R_], out=rst, in_=tokssq, func=AF.Sqrt, bias=1024.0 * EPS, scale=1.0)
        V("reciprocal", [R_], [R_], out=rst, in_=rst)
        lg3, pm3, tq3, e13, e23 = [v3(a_, NCH) for a_ in (lg, pm, tq, e1, e2)]
        V("tensor_tensor", ["ps4", R_], [R_], out=lg3, in0=v3(ps[4][:, 0:NG], NCH), in1=rst.unsqueeze(2).broadcast_to([128, NCH, 16]), op=ALU.mult)
        V("tensor_tensor", [R_], [R_], out=lg3[:, 0:2, :], in0=lg3[:, 0:2, :], in1=cbias[:, 16:32].unsqueeze(1).broadcast_to([128, 2, 16]), op=ALU.add)
        V("tensor_tensor", [R_], [R_], out=lg3[:, 2:NCH, :], in0=lg3[:, 2:NCH, :], in1=cbias[:, 0:16].unsqueeze(1).broadcast_to([128, NCH - 2, 16]), op=ALU.add)

        def sm(i, w_=1):
            return sm2[:, i * NCH * 4:i * NCH * 4 + NCH * w_]

        def rmax(dst, src3, width):
            V("tensor_tensor", [R_], [R_], out=dst, in0=src3[:, :, 0], in1=src3[:, :, 1], op=ALU.max)
            for i in range(2, width):
                V("tensor_tensor", [R_], [R_], out=dst, in0=dst, in1=src3[:, :, i], op=ALU.max)

        mx = sm(0)
        rmax(mx, lg3, 16)
        V("tensor_tensor", [R_], [R_], out=pm3, in0=lg3, in1=mx.unsqueeze(2).broadcast_to([128, NCH, 16]), op=ALU.subtract)
        A([R_], [R_], out=pm, in_=pm, func=AF.Exp)
        p4 = pm.rearrange("p (c g j) -> p c g j", c=NCH, g=4)
        gs = sm(1, 4)
        gs3 = v3(gs, NCH)
        pair = sm(2, 4)
        pair3 = v3(pair, NCH)
        first = True
        for a_ in range(4):
            for b_ in range(a_ + 1, 4):
                if first:
                    V("tensor_tensor", [R_], [R_], out=gs3, in0=p4[:, :, :, a_], in1=p4[:, :, :, b_], op=ALU.add)
                    first = False
                else:
                    V("tensor_tensor", [R_], [R_], out=pair3, in0=p4[:, :, :, a_], in1=p4[:, :, :, b_], op=ALU.add)
                    V("tensor_tensor", [R_], [R_], out=gs3, in0=gs3, in1=pair3, op=ALU.max)
        gmx = sm(3)
        rmax(gmx, gs3, 4)
        eq = sm(4, 4)
        eq3 = v3(eq, NCH)
        V("tensor_tensor", [R_], [R_], out=eq3, in0=gs3, in1=gmx.unsqueeze(2).broadcast_to([128, NCH, 4]), op=ALU.is_equal)
        rem = sm(5)
        selg = sm(6, 4)
        selg3 = v3(selg, NCH)
        V("tensor_copy", [R_], [R_], out=selg3[:, :, 0], in_=eq3[:, :, 0])
        V("tensor_scalar", [R_], [R_], out=rem, in0=eq3[:, :, 0], scalar1=-1.0, scalar2=1.0, op0=ALU.mult, op1=ALU.add)
        for g_ in range(1, 4):
            V("tensor_tensor", [R_], [R_], out=selg3[:, :, g_], in0=eq3[:, :, g_], in1=rem, op=ALU.mult)
            if g_ < 3:
                V("tensor_tensor", [R_], [R_], out=rem, in0=rem, in1=selg3[:, :, g_], op=ALU.subtract)
        V("tensor_tensor", [R_], [R_], out=p4, in0=p4, in1=selg3.unsqueeze(3).broadcast_to([128, NCH, 4, 4]), op=ALU.mult)
        v1 = sm(7)
        rmax(v1, pm3, 16)
        V("tensor_tensor", [R_], [R_], out=e13, in0=pm3, in1=v1.unsqueeze(2).broadcast_to([128, NCH, 16]), op=ALU.is_equal)
        V("tensor_tensor", [R_], [R_], out=tq3, in0=e13, in1=pm3, op=ALU.mult)
        V("tensor_tensor", [R_], [R_], out=tq3, in0=pm3, in1=tq3, op=ALU.subtract)
        v2 = sm(8)
        rmax(v2, tq3, 16)
        V("tensor_tensor", [R_], [R_], out=e23, in0=tq3, in1=v2.unsqueeze(2).broadcast_to([128, NCH, 16]), op=ALU.is_equal)
        V("tensor_tensor", [R_], [R_], out=e13, in0=e13, in1=e23, op=ALU.add)
        V("tensor_tensor", [R_], [R_], out=e13, in0=e13, in1=pm3, op=ALU.mult)
        V("tensor_tensor", [R_], [R_], out=v1, in0=v1, in1=v2, op=ALU.add)
        V("reciprocal", [R_], [R_], out=v1, in_=v1)
        V("tensor_tensor", [R_], [R_], out=e13, in0=e13, in1=v1.unsqueeze(2).broadcast_to([128, NCH, 16]), op=ALU.mult)
        for grp in range(5):
            cs_ = list(range(grp * 4, min(NCH, grp * 4 + 4)))
            TR([dict(out=ps[5][0:16, i * 128:(i + 1) * 128], in_=e13[:, c, :], ident=ident_f) for i, c in enumerate(cs_)],
               [R_, "consts"], ["ps5"])
            n_ = len(cs_) * 128
            A(["ps5"], ["GTb"], out=GTb[0:16, grp * 512:grp * 512 + n_], in_=ps[5][0:16, 0:n_], func=AF.Copy)
        V("tensor_copy", ["consts"], ["selE"], out=v3(selE, 16)[0:16], in_=ident_f[0:16, 0:16].unsqueeze(2).broadcast_to([16, 16, 128]))
        if "gates" in B.dbg and l == 0:
            B.dump("gates", e1, NG, [R_])
        t.barrier()
        if stop == "router":
            break

        regMB[1] = mark
        wexp = B.wexp_ap()
        wsl = [B.alloc(regMB, 3 * 4096, BF16) for _ in range(2)]
        GBb = [B.alloc(regMB, T, BF16) for _ in range(2)]
        actb = [B.alloc(regMB, 4 * 512, BF16) for _ in range(2)]
        silb = [B.alloc(regMB, 512) for _ in range(2)]
        ui = 0
        ci = 0
        pend = None
        item = 0

        def emit_down(pd_):
            Wd3_, act3_, apk, wk_, n0_, n1_, col_ = pd_
            n_ = n1_ - n0_
            for dch in range(8):
                pdb = 4 + (dch % 2)
                MM([dict(out=ps[pdb][:, 0:n_], lhsT=Wd3_[:, m, dch * 128:(dch + 1) * 128], rhs=act3_[:, m, 0:n_], start=(m == 0), stop=(m == 3))
                    for m in range(4)], [wk_, apk], [f"ps{pdb}"])
                V("scalar_tensor_tensor", [f"ps{pdb}", "modT", f"xT{dch}"], [f"xT{dch}"], out=x3[:, dch, n0_:n1_], in0=ps[pdb][:, 0:n_],
                  scalar=modc(l, 40 + dch, col_), in1=x3[:, dch, n0_:n1_], op0=ALU.mult, op1=ALU.add)

        for e in range(16):
            gp = e % 2
            for ti, (n0, n1) in enumerate(NT_l):
                n = n1 - n0
                MM([dict(out=ps[7][:, 0:n], lhsT=selE[0:16, e * 128:(e + 1) * 128], rhs=GTb[0:16, n0:n1])], ["selE", "GTb"], ["ps7"])
                A(["ps7"], [f"GB{gp}"], out=GBb[gp][:, n0:n1], in_=ps[7][:, 0:n], func=AF.Copy)
            for q in range(2):
                s = ui % 2
                ui += 1
                for i in range(3):
                    t.dma("pool", wsl[s][:, i * 4096:(i + 1) * 4096], wexp[l * 32 + e * 2 + q][:, i * 4096:(i + 1) * 4096],
                          w=[f"wx{s}_{i}"])
                Wg3, Wu3, Wd3 = v3(wsl[s][:, 0:4096], 8), v3(wsl[s][:, 4096:8192], 8), v3(wsl[s][:, 8192:12288], 4)
                for ti, (n0, n1) in enumerate(NT_l):
                    n = n1 - n0
                    col = 1 if n0 < TC else 0
                    pa_ = item % 2
                    item += 1
                    act3 = v3(actb[pa_], 4)
                    for m in range(4):
                        pg, pu = ci % 2, 2 + (ci % 2)
                        sl = silb[ci % 2]
                        sk = f"sil{ci % 2}"
                        ci += 1
                        MM([dict(out=ps[pg][:, 0:n], lhsT=Wg3[:, kc, m * 128:(m + 1) * 128], rhs=hT3[:, kc, n0:n1], start=(kc == 0), stop=(kc == 7))
                            for kc in range(8)], [f"wx{s}_0"] + HK, [f"ps{pg}"])
                        MM([dict(out=ps[pu][:, 0:n], lhsT=Wu3[:, kc, m * 128:(m + 1) * 128], rhs=hT3[:, kc, n0:n1], start=(kc == 0), stop=(kc == 7))
                            for kc in range(8)], [f"wx{s}_1"] + HK, [f"ps{pu}"])
                        A([f"ps{pg}"], [sk], out=sl[:, 0:n], in_=ps[pg][:, 0:n], func=AF.Silu)
                        V("tensor_tensor", [sk, f"ps{pu}"], [sk], out=sl[:, 0:n], in0=sl[:, 0:n], in1=ps[pu][:, 0:n], op=ALU.mult)
                        V("tensor_tensor", [sk, f"GB{gp}"], [f"act{pa_}"], out=act3[:, m, 0:n], in0=sl[:, 0:n], in1=GBb[gp][:, n0:n1], op=ALU.mult)
                    if pend is not None:
                        emit_down(pend)
                    pend = (Wd3, act3, f"act{pa_}", f"wx{s}_2", n0, n1, col)
        emit_down(pend)
        if "xend" in B.dbg and l == 0:
            B.dump("xend", xT, 8 * T, XK)
        t.barrier()

    if stop is None:
        reset(regMB)
        sqb = B.alloc(regMB, 8 * 512, BF16)
        sqb3 = v3(sqb, 8)
        rsf = [B.alloc(regMB, 512) for _ in range(2)]
        ob = [B.alloc(regMB, 512) for _ in range(4)]
        gf = B.alloc(regMB, 8)
        V("tensor_scalar", ["pvec"], ["gf"], out=gf, in0=pv(0, "g_final", 0, 8), scalar1=32.0, scalar2=None, op0=ALU.mult)
        oi = 0
        for ti, (n0, n1) in enumerate(NTILES[1:]):
            n = n1 - n0
            for k in range(8):
                A([f"xT{k}"], [f"sqb{k}"], out=sqb3[:, k, 0:n], in_=x3[:, k, n0:n1], func=AF.Square)
            MM([dict(out=ps[1][:, 0:n], lhsT=ones_b, rhs=sqb3[:, k, 0:n], start=(k == 0), stop=(k == 7)) for k in range(8)],
               [f"sqb{k}" for k in range(8)] + ["cb"], ["ps1"])
            r_ = rsf[ti % 2]
            rk = f"rsf{ti % 2}"
            A(["ps1"], [rk], out=r_[:, 0:n], in_=ps[1][:, 0:n], func=AF.Sqrt, bias=1024.0 * EPS, scale=1.0)
            V("reciprocal", [rk], [rk], out=r_[:, 0:n], in_=r_[:, 0:n])
            for k in range(8):
                o_ = ob[oi % 4]
                ok = f"ob{oi % 4}"
                oi += 1
                V("scalar_tensor_tensor", [f"xT{k}", rk, "gf"], [ok], out=o_[:, 0:n], in0=x3[:, k, n0:n1], scalar=gf[:, k:k + 1],
                  in1=r_[:, 0:n], op0=ALU.mult, op1=ALU.mult)
                t.dma("sp", out_d[:, k * 2048 + n0 - TC:k * 2048 + n1 - TC], o_[:, 0:n], r=[ok], w=[], sem=f"d_ob{oi % 4}")
    t.barrier()
    return B


def _fm(a):
    n = a.shape[0]
    return np.ascontiguousarray(a.T.reshape(8, 128, n).transpose(1, 0, 2).reshape(128, 8 * n))


def _cols(v, nt):
    return np.asarray(v, np.float32).reshape(nt, 128).T


def prep_shared(inp):
    f = lambda k: np.asarray(inp[k], np.float32)
    sh = {}
    w_ada = f("w_ada")
    sh["wada"] = np.ascontiguousarray(
        w_ada.reshape(NL, 8, 128, 12, 512).transpose(0, 3, 2, 1, 4).reshape(NL * 12, 128, 8 * 512))
    pvec = np.zeros((128, NL, NPV), np.float32)
    pbc = np.zeros((128, NL, NPB), np.float32)
    for l in range(NL):
        def put(name, arr):
            arr = np.asarray(arr, np.float32)
            pvec[:, l, PV[name]:PV[name] + arr.shape[1]] = arr
        put("b_ada", _cols(f("b_ada")[l], 48))
        put("g_mix", _cols(f("g_mix")[l], 8))
        put("g_ffn", _cols(f("g_ffn")[l], 8))
        cw = f("ssd_conv_w")[l]
        put("conv_w", np.concatenate([_cols(cw[k], 8) for k in range(5)], axis=1))
        put("conv_b", _cols(f("ssd_conv_b")[l], 8))
        put("ssd_d", _cols(np.repeat(f("ssd_d")[l], 64), 4))
        put("ssd_ng", _cols(f("ssd_norm_g")[l], 4))
        dw = f("conf_dw_w")[l]
        put("dw_w", np.concatenate([_cols(dw[k], 2) for k in range(31)], axis=1))
        put("dw_b", _cols(f("conf_dw_b")[l], 2))
        put("ln_g", _cols(f("conf_ln_g")[l], 2))
        put("ln_b", _cols(f("conf_ln_b")[l], 2))
        put("pw_b", _cols(f("conf_pw_b")[l], 2))
        put("s5_d", _cols(f("s5_d")[l], 2))
        put("glu_b", _cols(f("s5_glu_b")[l], 4))
        put("g_final", _cols(f("g_final"), 8))
        put("lamre", np.concatenate([_cols(f("s5_lambda_re")[l, d].reshape(-1), 8) for d in range(2)], axis=1))
        put("lamim", np.concatenate([_cols(f("s5_lambda_im")[l, d].reshape(-1), 8) for d in range(2)], axis=1))
        put("lstep", np.concatenate([_cols(np.repeat(f("s5_log_step")[l, d], 64), 8) for d in range(2)], axis=1))
        put("bre", f("s5_b_re")[l].reshape(8, 128, 16).transpose(1, 0, 2).reshape(128, 128))
        put("bim", f("s5_b_im")[l].reshape(8, 128, 16).transpose(1, 0, 2).reshape(128, 128))
        put("cre", f("s5_c_re")[l].transpose(0, 2, 1).reshape(8, 128, 16).transpose(1, 0, 2).reshape(128, 128))
        put("cim", f("s5_c_im")[l].transpose(0, 2, 1).reshape(8, 128, 16).transpose(1, 0, 2).reshape(128, 128))
        pbc[:, l, 0:16] = f("ssd_dt_bias")[l].reshape(1, 16)
        pbc[:, l, 16:32] = f("ssd_a_log")[l].reshape(1, 16)
        pbc[:, l, 32:48] = f("router_b").reshape(1, 16)
    sh["pvec"] = pvec.reshape(128, NL * NPV)
    sh["pbc"] = pbc.reshape(128, NL * NPB)
    k = np.arange(128)
    le = (k[:, None] <= k[None, :]).astype(np.float32)
    sh["consts"] = np.concatenate([np.eye(128, dtype=np.float32), np.ones((128, 128), np.float32), le, 1 - le,
                                   (k[:, None] >= k[None, :]).astype(np.float32),
                                   (k[:, None] < k[None, :]).astype(np.float32)], axis=1)
    w_in = f("w_in")
    win = np.zeros((NL, 19, 128, 8, 128), np.float32)
    for j, (c0, n) in enumerate(WIN_TILES):
        win[:, j, :, :, :n] = w_in[:, :, c0:c0 + n].reshape(NL, 8, 128, n).transpose(0, 2, 1, 3)
    sh["win"] = win.reshape(NL * 19, 128, 8 * 128)
    sh["rw"] = np.ascontiguousarray(f("router_w").reshape(8, 128, 16).transpose(1, 0, 2).reshape(128, 128))
    sh["wpw"] = np.ascontiguousarray(f("conf_pw_w").reshape(NL, 2, 128, 256).transpose(0, 2, 1, 3).reshape(NL, 128, 512))
    sh["wglu"] = np.ascontiguousarray(f("s5_glu_w").reshape(NL, 2, 128, 512).transpose(0, 2, 1, 3).reshape(NL, 128, 1024))
    sh["wout"] = np.ascontiguousarray(f("w_out").reshape(NL, 8, 128, 1024).transpose(0, 2, 1, 3).reshape(NL, 128, 8192))
    return sh


def prep_experts(inp):
    wexp = np.empty((NL, 16, 2, 128, 3, 4096), np.float32)
    for i, name in enumerate(("exp_w_gate", "exp_w_up")):
        w = np.asarray(inp[name], np.float32).reshape(NL, 16, 8, 128, 2, 512)
        wexp[:, :, :, :, i, :] = w.transpose(0, 1, 4, 3, 2, 5).reshape(NL, 16, 2, 128, 4096)
    w = np.asarray(inp["exp_w_down"], np.float32).reshape(NL, 16, 2, 4, 128, 1024)
    wexp[:, :, :, :, 2, :] = w.transpose(0, 1, 2, 4, 3, 5).reshape(NL, 16, 2, 128, 4096)
    return wexp.reshape(NL * 32, 128, 3 * 4096)


def prep_core(inp, b):
    xc = np.concatenate([np.asarray(inp["ctx"][b], np.float32), np.asarray(inp["x"][b], np.float32)], axis=0)
    cond = np.stack([_cols(inp["c"][b], 8), _cols(inp["c_ctx"], 8)], axis=2).reshape(128, 16)
    return {"xin": _fm(xc), "cond": np.ascontiguousarray(cond, dtype=np.float32)}


def kernel(**inputs):
    nc = bass.Bass("TRN2", target_bir_lowering=False)
    with contextlib.ExitStack() as es:
        build(nc, es)
    sh = prep_shared(inputs)
    sh["wexp"] = prep_experts(inputs)
    in_maps = []
    for b in range(8):
        m = dict(sh)
        m.update(prep_core(inputs, b))
        in_maps.append(m)
    res = run_bass_kernel_spmd(nc, in_maps, core_ids=list(range(8)))
    outs = [r["out"].reshape(128, 8, 2048).transpose(2, 1, 0).reshape(2048, 1024) for r in res.results]
    return np.stack(outs, axis=0).astype(np.float32)
```

```python
import contextlib
import math
import numpy as np
import concourse.bass as bass
import concourse.mybir as mybir
from concourse.bass_utils import run_bass_kernel_spmd

F32 = mybir.dt.float32
BF16 = mybir.dt.bfloat16
ALU = mybir.AluOpType
AF = mybir.ActivationFunctionType

NL = 2
T = 2304
TC = 256
NCH = 18
EPS = 1e-6
SQ = 8
NSC = T // SQ
NLEV = 9

PV = {}
_o = 0
for _n, _w in (("b_ada", 48), ("g_mix", 8), ("g_ffn", 8), ("conv_w", 40), ("conv_b", 8), ("ssd_d", 4),
               ("ssd_ng", 4), ("dw_w", 62), ("dw_b", 2), ("ln_g", 2), ("ln_b", 2), ("pw_b", 2), ("s5_d", 2),
               ("glu_b", 4), ("g_final", 8), ("lamre", 16), ("lamim", 16), ("lstep", 16),
               ("bre", 128), ("bim", 128), ("cre", 128), ("cim", 128)):
    PV[_n] = _o
    _o += _w
NPV = _o
NPB = 48
WIN_TILES = [(i * 128, 128) for i in range(12)] + [(1536, 16)] + [(1552 + i * 128, 128) for i in range(4)] + \
            [(2064 + i * 128, 128) for i in range(2)]
J_Z, J_XBC, J_DT, J_CONF, J_S5 = 0, 4, 12, 13, 17

NTILES = [(0, 256)] + [(256 + 512 * i, 256 + 512 * (i + 1)) for i in range(4)]


class Tracker:
    def __init__(self, nc, es):
        self.nc = nc
        self.es = es
        self.eng = dict(pe=nc.tensor, dve=nc.vector, act=nc.scalar, pool=nc.gpsimd, sp=nc.sync)
        self.semh = {}
        self.cnt = {}
        self.seen = {e: {} for e in self.eng}
        self.st = {}
        self.nins = 0

    def sem(self, name):
        if name not in self.semh:
            self.semh[name] = self.es.enter_context(self.nc.semaphore(name))
            self.cnt[name] = 0
        return self.semh[name]

    def _wait(self, e, toks):
        eng = self.eng[e]
        seen = self.seen[e]
        for (s, v) in toks:
            if seen.get(s, 0) < v:
                eng.wait_ge(self.semh[s], v)
                seen[s] = v

    def _deps(self, r, w):
        toks = []
        for k in r:
            st = self.st.get(k)
            if st and st[0]:
                toks.append(st[0])
        for k in w:
            st = self.st.get(k)
            if st:
                if st[0]:
                    toks.append(st[0])
                toks.extend(st[1].items())
        return toks

    def _record(self, tok, r, w):
        for k in r:
            st = self.st.setdefault(k, [None, {}])
            if st[1].get(tok[0], 0) < tok[1]:
                st[1][tok[0]] = tok[1]
        for k in w:
            self.st[k] = [tok, {}]

    def op(self, e, fns, r=(), w=()):
        self._wait(e, self._deps(r, w))
        if callable(fns):
            fns = [fns]
        ins = None
        for f in fns:
            ins = f()
            self.nins += 1
        s = "s_" + e
        self.sem(s)
        ins.then_inc(self.semh[s], 1)
        self.cnt[s] += 1
        tok = (s, self.cnt[s])
        self._record(tok, r, w)
        return tok

    def dma(self, q, out, in_, r=(), w=(), sem=None):
        self._wait(q, self._deps(r, w))
        s = sem or ("d_" + (w[0] if w else r[0]))
        self.sem(s)
        self.eng[q].dma_start(out=out, in_=in_).then_inc(self.semh[s], 16)
        self.cnt[s] += 16
        tok = (s, self.cnt[s])
        self._record(tok, r, w)
        self.nins += 1
        return tok

    def barrier(self):
        toks = [(s, v) for s, v in self.cnt.items() if v > 0]
        for e in self.eng:
            self._wait(e, toks)
        self.st = {}


class Builder:
    def __init__(self, nc, es, dbg=None, stop=None, nlayers=NL):
        self.nc = nc
        self.es = es
        self.t = Tracker(nc, es)
        self.dbg = dbg or []
        self.stop = stop
        self.nlayers = nlayers
        self.AW = 53200
        self.arena = es.enter_context(nc.sbuf_tensor("arena", [128, self.AW], F32))
        self.ps = [es.enter_context(nc.psum_tensor(f"psb{i}", [128, 512], F32)) for i in range(8)]
        self.dumps = []
        self.uid = 0

    def region(self, start_words, nwords):
        return [start_words, start_words, start_words + nwords]

    def alloc(self, reg, n, dtype=F32):
        words = n if dtype == F32 else (n + 1) // 2
        words = (words + 7) // 8 * 8
        o = reg[1]
        assert o + words <= reg[2], f"arena region overflow: need {words} have {reg[2] - o}"
        reg[1] = o + words
        ap = self.arena[:, o:o + (n if dtype == F32 else (n + 1) // 2)]
        if dtype != F32:
            ap = ap.bitcast(dtype)
        return ap

    def key(self, base):
        self.uid += 1
        return f"{base}#{self.uid}"

    def V(self, name, r, w, **kw):
        return self.t.op("dve", lambda: getattr(self.nc.vector, name)(**kw), r, w)

    def A(self, r, w, **kw):
        return self.t.op("act", lambda: self.nc.scalar.activation(**kw), r, w)

    def G(self, name, r, w, **kw):
        return self.t.op("pool", lambda: getattr(self.nc.gpsimd, name)(**kw), r, w)

    def MM(self, mms, r, w):
        fns = [(lambda kw=kw: self.nc.tensor.matmul(kw["out"], lhsT=kw["lhsT"], rhs=kw["rhs"],
                                                    start=kw.get("start", True), stop=kw.get("stop", True)))
               for kw in mms]
        return self.t.op("pe", fns, r, w)

    def TR(self, trs, r, w):
        fns = [(lambda kw=kw: self.nc.tensor.transpose(kw["out"], kw["in_"], kw["ident"])) for kw in trs]
        return self.t.op("pe", fns, r, w)

    def wexp_ap(self):
        if not hasattr(self, "_wexp"):
            self._wexp = self.nc.dram_tensor("wexp", [NL * 32, 128, 3 * 4096], F32, kind="ExternalInput").ap()
            self.uses_moe = True
        return self._wexp

    def dump(self, name, ap2d, n, r):
        d = self.nc.dram_tensor("dbg_" + name, [128, n], F32, kind="ExternalOutput").ap()
        self.t.dma("sp", d, ap2d, r=r, w=[], sem="d_out")

    def dump_any(self, name, ap2d, n, r, dtype):
        if name not in self.dbg:
            return
        if dtype == F32:
            self.dump(name, ap2d, n, r)
        else:
            tmp = self.alloc(self.regD, n, F32)
            k = self.key("dbgtmp")
            self.V("tensor_copy", r, [k], out=tmp, in_=ap2d)
            self.dump(name, tmp, n, [k])


def v3(ap, a):
    return ap.rearrange("p (a b) -> p a b", a=a)


def v4(ap, a, b):
    return ap.rearrange("p (a b c) -> p a b c", a=a, b=b)


def build(nc, es, dbg=None, stop=None, nlayers=NL):
    B = Builder(nc, es, dbg, stop, nlayers)
    t = B.t
    V, A, G, MM, TR = B.V, B.A, B.G, B.MM, B.TR
    ps = B.ps

    def din(name, shape):
        return nc.dram_tensor(name, shape, F32, kind="ExternalInput").ap()

    xin = din("xin", [128, 8 * T])
    cond = din("cond", [128, 16])
    wada = din("wada", [NL * 12, 128, 8 * 512])
    pvec_d = din("pvec", [128, NL * NPV])
    pbc_d = din("pbc", [128, NL * NPB])
    consts_d = din("consts", [128, 6 * 128])
    win = din("win", [NL * 19, 128, 8 * 128])
    rw_d = din("rw", [128, 8 * 16])
    wpw_d = din("wpw", [NL, 128, 2 * 256])
    wglu_d = din("wglu", [NL, 128, 2 * 512])
    wout_d = din("wout", [NL, 128, 8 * 1024])
    out_d = nc.dram_tensor("out", [128, 8 * 2048], F32, kind="ExternalOutput").ap()
    xsp = nc.dram_tensor("xsp", [128, 8 * T], F32, kind="Internal").ap()

    P0 = B.region(0, 4608)
    HT0 = 4608
    HTW = 8 * T // 2
    R0 = HT0 + HTW
    AW = 8 * T
    MW = 4 * T // 2
    regA = B.region(R0, AW)
    regM = B.region(R0 + AW, MW)
    regB = B.region(R0 + AW + MW, B.AW - (R0 + AW + MW))
    regMB = B.region(R0 + AW, B.AW - (R0 + AW))
    B.regD = B.region(0, 0)

    def reset(reg):
        reg[1] = reg[0]

    hT = B.arena[:, HT0:HT0 + HTW].bitcast(BF16)
    hT3 = v3(hT, 8)
    xT = B.arena[:, R0:R0 + AW]
    x3 = v3(xT, 8)
    mixbs = B.arena[:, R0 + AW:R0 + AW + MW].bitcast(BF16)
    mix3 = v3(mixbs, 4)

    consts = B.alloc(P0, 6 * 128)
    ident_f, ones_f, LE_f, GT_f, GE_f, LT_f = [consts[:, i * 128:(i + 1) * 128] for i in range(6)]
    cb = B.alloc(P0, 2 * 128, BF16)
    ident_b, ones_b = cb[:, 0:128], cb[:, 128:256]
    pvec = B.alloc(P0, NL * NPV)
    pbc = B.alloc(P0, NL * NPB)
    condS = B.alloc(P0, 16)
    modT = B.alloc(P0, NL * 96)
    der = B.alloc(P0, NL * 32)
    rwf = B.alloc(P0, 128)

    def pv(l, name, c=0, n=1):
        o = l * NPV + PV[name] + c
        return pvec[:, o:o + n]

    def modc(l, j, col):
        o = l * 96 + j * 2 + col
        return modT[:, o:o + 1]

    def derc(l, which, k, col):
        o = l * 32 + which * 16 + k * 2 + col
        return der[:, o:o + 1]

    t.dma("sp", consts, consts_d, w=["consts"])
    t.dma("sp", pvec, pvec_d, w=["pvec"])
    t.dma("sp", pbc, pbc_d, w=["pbc"])
    t.dma("sp", condS, cond, w=["cond"])
    t.dma("sp", rwf, rw_d, w=["rwf"])
    V("tensor_copy", ["consts"], ["cb"], out=cb, in_=consts[:, 0:256])
    A(["cond"], ["condS"], out=condS, in_=condS, func=AF.Silu)
    for k in range(8):
        t.dma("sp", x3[:, k, :], xin[:, k * T:(k + 1) * T], w=[f"xT{k}"])
    reset(regB)
    pidx = B.alloc(regB, 8)
    invf = B.alloc(regB, 8)
    iof = B.alloc(regB, 64)
    ang = B.alloc(regB, 128)
    sn_, cs__, cc_, ss_ = [B.alloc(regB, 128) for _ in range(4)]
    PE_ = "posemb"
    V("reduce_sum", ["consts"], [PE_], out=pidx[:, 0:1], in_=LE_f, axis=mybir.AxisListType.X)
    V("tensor_scalar", [PE_], [PE_], out=pidx[:, 0:1], in0=pidx[:, 0:1], scalar1=-1.0, scalar2=128.0, op0=ALU.mult, op1=ALU.add)
    V("tensor_scalar", [PE_], [PE_], out=pidx[:, 1:2], in0=pidx[:, 0:1], scalar1=128.0, scalar2=None, op0=ALU.add)
    A([PE_], [PE_], out=invf[:, 0:2], in_=pidx[:, 0:2], func=AF.Exp, scale=-math.log(10000.0) / 256.0)
    MM([dict(out=ps[3][:, 0:128], lhsT=ones_f, rhs=LT_f)], ["consts"], ["ps3"])
    V("tensor_copy", ["ps3"], [PE_], out=iof, in_=ps[3][:, 0:64])
    V("tensor_tensor", [PE_], [PE_], out=v3(ang, 2), in0=iof.unsqueeze(1).broadcast_to([128, 2, 64]),
      in1=invf[:, 0:2].unsqueeze(2).broadcast_to([128, 2, 64]), op=ALU.mult)
    A([PE_], [PE_], out=sn_, in_=ang, func=AF.Sin, scale=1.0 / 64.0)
    A([PE_], [PE_], out=cs__, in_=ang, func=AF.Sin, scale=1.0 / 64.0, bias=math.pi / 2)
    for _ in range(6):
        V("tensor_tensor", [PE_], [PE_], out=cc_, in0=cs__, in1=cs__, op=ALU.mult)
        V("tensor_tensor", [PE_], [PE_], out=ss_, in0=sn_, in1=sn_, op=ALU.mult)
        V("scalar_tensor_tensor", [PE_], [PE_], out=sn_, in0=cs__, scalar=2.0, in1=sn_, op0=ALU.mult, op1=ALU.mult)
        V("tensor_tensor", [PE_], [PE_], out=cs__, in0=cc_, in1=ss_, op=ALU.subtract)
    for k in range(8):
        tab = v3(sn_ if (k // 2) % 2 == 0 else cs__, 2)[:, k % 2, :]
        xl = x3[:, k, TC:].rearrange("p (r c) -> p r c", c=64)
        if k < 4:
            in1 = tab[:, 0:32].unsqueeze(2).broadcast_to([128, 32, 64])
        else:
            in1 = tab.unsqueeze(1).broadcast_to([128, 32, 64])
        V("tensor_tensor", [PE_, f"xT{k}"], [f"xT{k}"], out=xl, in0=xl, in1=in1, op=ALU.add)
    t.barrier()

    reset(regMB)
    wslots = [B.alloc(regMB, 8 * 512) for _ in range(2)]
    modrow = B.alloc(regMB, 6144)
    condS3 = v3(condS, 8)
    gi = 0
    for l in range(nlayers):
        for blk in range(12):
            s = gi % 2
            gi += 1
            t.dma("sp", wslots[s], wada[l * 12 + blk], w=[f"wada{s}"], sem=f"d_wada{s}")
            pi = 1 + s
            MM([dict(out=ps[pi][0:2, 0:512], lhsT=condS3[:, kc, :], rhs=wslots[s][:, kc * 512:(kc + 1) * 512],
                     start=(kc == 0), stop=(kc == 7)) for kc in range(8)], [f"wada{s}", "condS"], [f"ps{pi}"])
            A([f"ps{pi}"], ["modrow"], out=modrow[0:2, blk * 512:(blk + 1) * 512], in_=ps[pi][0:2, 0:512], func=AF.Copy)
        TR([dict(out=ps[0][:, l * 96 + j * 2: l * 96 + j * 2 + 2], in_=modrow[0:2, j * 128:(j + 1) * 128], ident=ident_f[0:2, 0:2])
            for j in range(48)], ["modrow", "consts"], ["ps0"])
        V("tensor_tensor", ["ps0", "pvec"], ["modT"], out=v3(modT[:, l * 96:(l + 1) * 96], 48),
          in0=v3(ps[0][:, l * 96:(l + 1) * 96], 48),
          in1=pv(l, "b_ada", 0, 48).unsqueeze(2).broadcast_to([128, 48, 2]), op=ALU.add)
        for which, (gname, scj) in enumerate((("g_mix", 8), ("g_ffn", 32))):
            dst = v3(der[:, l * 32 + which * 16: l * 32 + which * 16 + 16], 8)
            V("tensor_scalar", ["modT"], ["der"], out=dst, in0=v3(modT[:, l * 96 + scj * 2: l * 96 + scj * 2 + 16], 8),
              scalar1=1.0, scalar2=32.0, op0=ALU.add, op1=ALU.mult)
            V("tensor_tensor", ["der", "pvec"], ["der"], out=dst, in0=dst,
              in1=pv(l, gname, 0, 8).unsqueeze(2).broadcast_to([128, 8, 2]), op=ALU.mult)
    t.barrier()
    if "mod" in B.dbg:
        B.dump("mod", modT, NL * 96, ["modT"])
        B.dump("x0", xT, 8 * T, [f"xT{k}" for k in range(8)])

    XK = [f"xT{k}" for k in range(8)]

    def rmsnorm_to_h(l, which, reg, tok_ssq=None, ntiles=NTILES):
        shj = 0 if which == 0 else 24
        sqb = B.alloc(reg, 8 * 512, BF16)
        sqb3 = v3(sqb, 8)
        rs = [B.alloc(reg, 512) for _ in range(2)]
        yt = [B.alloc(reg, 512) for _ in range(2)]
        it = 0
        for ti, (n0, n1) in enumerate(ntiles):
            n = n1 - n0
            col = 1 if n0 < TC else 0
            for k in range(8):
                if k % 2 == 0:
                    A([f"xT{k}"], [f"sqb{k}"], out=sqb3[:, k, 0:n], in_=x3[:, k, n0:n1], func=AF.Square)
                else:
                    V("tensor_tensor", [f"xT{k}"], [f"sqb{k}"], out=sqb3[:, k, 0:n], in0=x3[:, k, n0:n1], in1=x3[:, k, n0:n1], op=ALU.mult)
            MM([dict(out=ps[1][:, 0:n], lhsT=ones_b, rhs=sqb3[:, k, 0:n], start=(k == 0), stop=(k == 7)) for k in range(8)],
               [f"sqb{k}" for k in range(8)] + ["cb"], ["ps1"])
            if tok_ssq is not None:
                for c in range(n // 128):
                    ch = (n0 + c * 128) // 128
                    MM([dict(out=ps[2][:, ch:ch + 1], lhsT=sqb3[:, k, c * 128:(c + 1) * 128], rhs=ones_b[:, 0:1],
                             start=(k == 0), stop=(k == 7)) for k in range(8)],
                       [f"sqb{k}" for k in range(8)] + ["cb"], ["ps2"])
            r_ = rs[ti % 2]
            rk = f"rs{ti % 2}"
            A(["ps1"], [rk], out=r_[:, 0:n], in_=ps[1][:, 0:n], func=AF.Sqrt, bias=1024.0 * EPS, scale=1.0)
            V("reciprocal", [rk], [rk], out=r_[:, 0:n], in_=r_[:, 0:n])
            for k in range(8):
                y_ = yt[it % 2]
                yk = f"yt{it % 2}"
                it += 1
                V("tensor_tensor", [f"xT{k}", rk], [yk], out=y_[:, 0:n], in0=x3[:, k, n0:n1], in1=r_[:, 0:n], op=ALU.mult)
                A([yk, "der", "modT"], [f"hT{k}"], out=hT3[:, k, n0:n1], in_=y_[:, 0:n], func=AF.Identity,
                  scale=derc(l, which, k, col), bias=modc(l, shj + k, col))
        if tok_ssq is not None:
            V("tensor_copy", ["ps2"], ["tokssq"], out=tok_ssq, in_=ps[2][:, 0:NCH])

    HK = [f"hT{k}" for k in range(8)]

    win_state = {"i": 0}

    def load_win_tile(l, j, reg_slots):
        s = win_state["i"] % len(reg_slots)
        win_state["i"] += 1
        t.dma("pool", reg_slots[s], win[l * 19 + j], w=[f"wins{s}"], sem=f"d_wins{s}")
        return v3(reg_slots[s], 8), f"wins{s}"

    def proj(wt, wk, n0, n1, pst, psk, m=128):
        MM([dict(out=pst[0:m, 0:n1 - n0], lhsT=wt[:, kc, 0:m], rhs=hT3[:, kc, n0:n1], start=(kc == 0), stop=(kc == 7))
            for kc in range(8)], [wk] + HK, [psk])

    for l in range(nlayers):
        last = (l == NL - 1)
        NT_l = NTILES[1:] if last else NTILES
        reset(regB)
        rmsnorm_to_h(l, 0, regB)
        for k in range(8):
            t.dma("sp", xsp[:, k * T:(k + 1) * T], x3[:, k, :], r=[f"xT{k}"], w=[f"xsp{k}"])
        if "h" in B.dbg and l == 0:
            reset(regA)
            B.regD = regA
            t.barrier()
            B.dump_any("h", hT, 8 * T, HK, BF16)
        t.barrier()
        if stop == "h":
            break

        reset(regA)
        reset(regB)
        wslots_in = [B.alloc(regB, 8 * 128, BF16) for _ in range(3)]
        uTb = B.alloc(regB, 2 * T, BF16)
        uT3 = v3(uTb, 2)
        for c in range(2):
            wt, wk = load_win_tile(l, J_S5 + c, wslots_in)
            for ti, (n0, n1) in enumerate(NTILES):
                pi = 3 + (ti % 2)
                proj(wt, wk, n0, n1, ps[pi], f"ps{pi}")
                A([f"ps{pi}"], [f"uT{c}"], out=uT3[:, c, n0:n1], in_=ps[pi][:, 0:n1 - n0], func=AF.Copy)
        UK = ["uT0", "uT1"]
        yacc = B.alloc(regB, 2 * T)
        yacc3 = v3(yacc, 2)
        V("memset", [], ["yacc"], ap=yacc, constant=0.0)
        Cz = [B.alloc(regB, 8 * 128, BF16) for _ in range(2)]
        for ri in range(2):
            V("memset", [], [f"Cz{ri}"], ap=Cz[ri], constant=0.0)
            src = v3(pv(l, "cre" if ri == 0 else "cim", 0, 128), 8)
            for m in range(4):
                for hf in range(2):
                    dst = v3(Cz[ri], 8)[hf * 64:(hf + 1) * 64, m::4, 32 * m + 16 * hf: 32 * m + 16 * hf + 16]
                    V("tensor_scalar", ["pvec"], [f"Cz{ri}"], out=dst, in0=src[hf * 64:(hf + 1) * 64, m::4, :],
                      scalar1=(1.0 if ri == 0 else -1.0), scalar2=None, op0=ALU.mult)
        sm = B.alloc(regB, 64 * 8)

        def smt(i):
            return sm[:, i * 8:(i + 1) * 8]

        PWr = B.alloc(regB, (SQ + 1) * 8)
        PWi = B.alloc(regB, (SQ + 1) * 8)
        NPi = B.alloc(regB, (SQ + 1) * 8)
        MUr = B.alloc(regB, NLEV * 8)
        MUi = B.alloc(regB, NLEV * 8)
        NMi = B.alloc(regB, NLEV * 8)
        Bb = [B.alloc(regB, 128) for _ in range(2)]
        Zp = B.alloc(regB, 8 * 128, BF16)
        BzT = [B.alloc(regB, 8 * 128, BF16) for _ in range(2)]
        bus = [B.alloc(regA, 2 * T, BF16) for _ in range(2)]
        dgs = [B.alloc(regA, 3 * (SQ + 1) * 128, BF16) for _ in range(3)]
        xbfs = [B.alloc(regA, 2 * NSC, BF16) for _ in range(2)]
        itd = 0
        cnt5 = {"ith": 0}
        hbs = [B.alloc(regA, 2 * T, BF16) for _ in range(2)]
        xas = [B.alloc(regA, 2 * NSC) for _ in range(2)]
        xbs = [B.alloc(regA, 2 * NSC) for _ in range(2)]
        S = "s5s"
        for d in range(2):
            lre, lim, lst = pv(l, "lamre", d * 8, 8), pv(l, "lamim", d * 8, 8), pv(l, "lstep", d * 8, 8)
            step, th, lrs, mag, sn, cs_, cc, ss, ar, ai, den, am1, cr, ci, tmp, tmp2 = [smt(i) for i in range(16)]
            A(["pvec", S], [S], out=step, in_=lst, func=AF.Exp)
            V("tensor_tensor", [S, "pvec"], [S], out=th, in0=lim, in1=step, op=ALU.mult)
            V("tensor_tensor", [S, "pvec"], [S], out=lrs, in0=lre, in1=step, op=ALU.mult)
            A([S], [S], out=mag, in_=lrs, func=AF.Exp)
            A([S], [S], out=sn, in_=th, func=AF.Sin, scale=1.0 / 16.0)
            A([S], [S], out=cs_, in_=th, func=AF.Sin, scale=1.0 / 16.0, bias=math.pi / 2)
            for _ in range(4):
                V("tensor_tensor", [S], [S], out=cc, in0=cs_, in1=cs_, op=ALU.mult)
                V("tensor_tensor", [S], [S], out=ss, in0=sn, in1=sn, op=ALU.mult)
                V("scalar_tensor_tensor", [S], [S], out=sn, in0=cs_, scalar=2.0, in1=sn, op0=ALU.mult, op1=ALU.mult)
                V("tensor_tensor", [S], [S], out=cs_, in0=cc, in1=ss, op=ALU.subtract)
            V("tensor_tensor", [S], [S], out=ar, in0=mag, in1=cs_, op=ALU.mult)
            V("tensor_tensor", [S], [S], out=ai, in0=mag, in1=sn, op=ALU.mult)
            V("tensor_tensor", [S, "pvec"], [S], out=den, in0=lre, in1=lre, op=ALU.mult)
            V("tensor_tensor", [S, "pvec"], [S], out=tmp, in0=lim, in1=lim, op=ALU.mult)
            V("tensor_tensor", [S], [S], out=den, in0=den, in1=tmp, op=ALU.add)
            V("reciprocal", [S], [S], out=den, in_=den)
            V("tensor_scalar", [S], [S], out=am1, in0=ar, scalar1=-1.0, scalar2=None, op0=ALU.add)
            V("tensor_tensor", [S, "pvec"], [S], out=tmp, in0=am1, in1=lre, op=ALU.mult)
            V("tensor_tensor", [S, "pvec"], [S], out=tmp2, in0=ai, in1=lim, op=ALU.mult)
            V("tensor_tensor", [S], [S], out=cr, in0=tmp, in1=tmp2, op=ALU.add)
            V("tensor_tensor", [S], [S], out=cr, in0=cr, in1=den, op=ALU.mult)
            V("tensor_tensor", [S, "pvec"], [S], out=tmp, in0=ai, in1=lre, op=ALU.mult)
            V("tensor_tensor", [S, "pvec"], [S], out=tmp2, in0=am1, in1=lim, op=ALU.mult)
            V("tensor_tensor", [S], [S], out=ci, in0=tmp, in1=tmp2, op=ALU.subtract)
            V("tensor_tensor", [S], [S], out=ci, in0=ci, in1=den, op=ALU.mult)
            bre3, bim3 = v3(pv(l, "bre", 0, 128), 8), v3(pv(l, "bim", 0, 128), 8)
            crb = cr.unsqueeze(2).broadcast_to([128, 8, 16])
            cib = ci.unsqueeze(2).broadcast_to([128, 8, 16])
            t1 = B.alloc(regB, 128) if d == 0 else t1
            V("tensor_tensor", [S, "pvec"], ["Bb0"], out=v3(Bb[0], 8), in0=bre3, in1=crb, op=ALU.mult)
            V("tensor_tensor", [S, "pvec"], ["t1"], out=v3(t1, 8), in0=bim3, in1=cib, op=ALU.mult)
            V("tensor_tensor", ["Bb0", "t1"], ["Bb0"], out=Bb[0], in0=Bb[0], in1=t1, op=ALU.subtract)
            V("tensor_tensor", [S, "pvec"], ["Bb1"], out=v3(Bb[1], 8), in0=bim3, in1=crb, op=ALU.mult)
            V("tensor_tensor", [S, "pvec", "Bb0"], ["t1"], out=v3(t1, 8), in0=bre3, in1=cib, op=ALU.mult)
            V("tensor_tensor", ["Bb1", "t1"], ["Bb1"], out=Bb[1], in0=Bb[1], in1=t1, op=ALU.add)
            for ri in range(2):
                V("memset", [], ["Zp"], ap=Zp, constant=0.0)
                for m in range(4):
                    for hf in range(2):
                        dst = v3(Zp, 8)[hf * 64:(hf + 1) * 64, m::4, 32 * m + 16 * hf: 32 * m + 16 * hf + 16]
                        V("tensor_copy", [f"Bb{ri}"], ["Zp"], out=dst, in_=v3(Bb[ri], 8)[hf * 64:(hf + 1) * 64, m::4, :])
                psb = ps[5][:, 0:512].bitcast(BF16)
                TR([dict(out=psb[:, tt * 128:(tt + 1) * 128], in_=Zp[:, tt * 128:(tt + 1) * 128], ident=ident_b)
                    for tt in range(8)], ["Zp", "cb"], ["ps5"])
                V("tensor_copy", ["ps5"], [f"BzT{ri}"], out=BzT[ri], in_=psb)
            V("memset", [], [S], ap=PWr[:, 0:8], constant=1.0)
            V("memset", [], [S], ap=PWi[:, 0:8], constant=0.0)
            for j in range(1, SQ + 1):
                pr0, pi0 = PWr[:, (j - 1) * 8:j * 8], PWi[:, (j - 1) * 8:j * 8]
                pr1, pi1 = PWr[:, j * 8:(j + 1) * 8], PWi[:, j * 8:(j + 1) * 8]
                V("tensor_tensor", [S], [S], out=tmp, in0=pi0, in1=ai, op=ALU.mult)
                V("tensor_tensor", [S], [S], out=pr1, in0=pr0, in1=ar, op=ALU.mult)
                V("tensor_tensor", [S], [S], out=pr1, in0=pr1, in1=tmp, op=ALU.subtract)
                V("tensor_tensor", [S], [S], out=tmp, in0=pi0, in1=ar, op=ALU.mult)
                V("tensor_tensor", [S], [S], out=pi1, in0=pr0, in1=ai, op=ALU.mult)
                V("tensor_tensor", [S], [S], out=pi1, in0=pi1, in1=tmp, op=ALU.add)
            V("tensor_scalar", [S], [S], out=NPi, in0=PWi, scalar1=-1.0, scalar2=None, op0=ALU.mult)
            V("tensor_copy", [S], [S], out=MUr[:, 0:8], in_=PWr[:, SQ * 8:(SQ + 1) * 8])
            V("tensor_copy", [S], [S], out=MUi[:, 0:8], in_=PWi[:, SQ * 8:(SQ + 1) * 8])
            for k in range(1, NLEV):
                mr0, mi0 = MUr[:, (k - 1) * 8:k * 8], MUi[:, (k - 1) * 8:k * 8]
                mr1, mi1 = MUr[:, k * 8:(k + 1) * 8], MUi[:, k * 8:(k + 1) * 8]
                V("tensor_tensor", [S], [S], out=tmp, in0=mi0, in1=mi0, op=ALU.mult)
                V("tensor_tensor", [S], [S], out=mr1, in0=mr0, in1=mr0, op=ALU.mult)
                V("tensor_tensor", [S], [S], out=mr1, in0=mr1, in1=tmp, op=ALU.subtract)
                V("scalar_tensor_tensor", [S], [S], out=mi1, in0=mr0, scalar=2.0, in1=mi0, op0=ALU.mult, op1=ALU.mult)
            V("tensor_scalar", [S], [S], out=NMi, in0=MUi, scalar1=-1.0, scalar2=None, op0=ALU.mult)

            def cmap(n0, n1):
                if d == 0:
                    return n0, n1
                if n0 < TC:
                    return 2048 + n0, 2048 + n1
                return n0 - TC, n1 - TC

            def build_diag(tt_):
                dg_ = dgs[tt_ % 3]
                for j in range(SQ + 1):
                    for v_, tab in enumerate((PWr, PWi, NPi)):
                        V("tensor_scalar", ["consts", S], [f"dg{tt_ % 3}"], out=dg_[:, (j * 3 + v_) * 128:(j * 3 + v_ + 1) * 128], in0=ident_f,
                          scalar1=tab[:, j * 8 + tt_: j * 8 + tt_ + 1], scalar2=None, op0=ALU.mult)

            def s5_iter(tt, dgp):
                hfc = tt // 4
                bu4, hb3 = v4(bus[dgp], 2, SQ), v3(hbs[dgp], 2)
                xa, xb_, xbf = xas[dgp], xbs[dgp], xbfs[dgp]
                BU, XK_, XBF, HB = f"bu{dgp}", f"x{dgp}", f"xbf{dgp}", f"hb{dgp}"
                for ti, (n0, n1) in enumerate(NTILES):
                    b0, b1 = cmap(n0, n1)
                    for ri in range(2):
                        pi = 4 + ((ti * 2 + ri) % 2)
                        MM([dict(out=ps[pi][:, 0:n1 - n0], lhsT=v3(BzT[ri], 8)[:, tt, :], rhs=uT3[:, hfc, n0:n1])],
                           [f"BzT{ri}", f"uT{hfc}"], [f"ps{pi}"])
                        A([f"ps{pi}"], [BU], out=bu4[:, ri, :, b0 // SQ:b1 // SQ],
                          in_=ps[pi][:, 0:n1 - n0].rearrange("p (c t) -> p t c", t=SQ), func=AF.Copy)

                def sc(tab, j):
                    return tab[:, j * 8 + tt: j * 8 + tt + 1]

                dg = dgs[tt % 3]
                dk = f"dg{tt % 3}"

                def dgm(j, v_):
                    return dg[:, (j * 3 + v_) * 128:(j * 3 + v_ + 1) * 128]

                def cterms(j, src_r, src_i, out_r, out_i, first, last):
                    mr = [dict(out=out_r, lhsT=dgm(j, 0), rhs=src_r, start=first, stop=False),
                          dict(out=out_r, lhsT=dgm(j, 2), rhs=src_i, start=False, stop=last)]
                    mi = [dict(out=out_i, lhsT=dgm(j, 1), rhs=src_r, start=first, stop=False),
                          dict(out=out_i, lhsT=dgm(j, 0), rhs=src_i, start=False, stop=last)]
                    return mr, mi

                mr_all, mi_all = [], []
                for s_ in range(SQ):
                    j = SQ - 1 - s_ if d == 0 else s_
                    mr, mi = cterms(j, bu4[:, 0, s_, :], bu4[:, 1, s_, :], ps[6][:, 0:NSC], ps[7][:, 0:NSC], s_ == 0, s_ == SQ - 1)
                    mr_all += mr
                    mi_all += mi
                MM(mr_all, [BU, dk], ["ps6"])
                MM(mi_all, [BU, dk], ["ps7"])
                xa3, xb3 = v3(xa, 2), v3(xb_, 2)
                A(["ps6"], [XK_], out=xa3[:, 0, :], in_=ps[6][:, 0:NSC], func=AF.Copy)
                A(["ps7"], [XK_], out=xa3[:, 1, :], in_=ps[7][:, 0:NSC], func=AF.Copy)
                if tt + 1 < 8:
                    build_diag(tt + 1)
                cur, nxt = xa3, xb3
                for k in range(NLEV):
                    s_ = 1 << k
                    V("tensor_copy", [XK_], [XK_], out=nxt, in_=cur)
                    if d == 0:
                        dr, di, sr, si = nxt[:, 0, s_:], nxt[:, 1, s_:], cur[:, 0, 0:NSC - s_], cur[:, 1, 0:NSC - s_]
                    else:
                        dr, di, sr, si = nxt[:, 0, 0:NSC - s_], nxt[:, 1, 0:NSC - s_], cur[:, 0, s_:], cur[:, 1, s_:]
                    for (o_, i_, scl) in ((dr, sr, sc(MUr, k)), (dr, si, sc(NMi, k)), (di, si, sc(MUr, k)), (di, sr, sc(MUi, k))):
                        V("scalar_tensor_tensor", [XK_, S], [XK_], out=o_, in0=i_, scalar=scl, in1=o_, op0=ALU.mult, op1=ALU.add)
                    cur, nxt = nxt, cur
                V("tensor_copy", [XK_], [XBF], out=xbf, in_=cur)
                xbf3 = v3(xbf, 2)

                def stage2():
                    for tau in range(SQ):
                        hp = cnt5["ith"] % 2
                        cnt5["ith"] += 1
                        o_r, o_i = ps[0 + hp][:, 0:NSC], ps[2 + hp][:, 0:NSC]
                        srcs = list(range(0, tau + 1)) if d == 0 else list(range(tau, SQ))
                        mr_all, mi_all = [], []
                        for n_, s_ in enumerate(srcs):
                            mr, mi = cterms(abs(tau - s_), bu4[:, 0, s_, :], bu4[:, 1, s_, :], o_r, o_i, n_ == 0, False)
                            mr_all += mr
                            mi_all += mi
                        if d == 0:
                            mr, mi = cterms(tau + 1, xbf3[:, 0, 0:NSC - 1], xbf3[:, 1, 0:NSC - 1], ps[0 + hp][:, 1:NSC], ps[2 + hp][:, 1:NSC], False, True)
                        else:
                            mr, mi = cterms(SQ - tau, xbf3[:, 0, 1:NSC], xbf3[:, 1, 1:NSC], ps[0 + hp][:, 0:NSC - 1], ps[2 + hp][:, 0:NSC - 1], False, True)
                        mr_all += mr
                        mi_all += mi
                        MM(mr_all, [BU, dk, XBF], [f"ps{0 + hp}"])
                        MM(mi_all, [BU, dk, XBF], [f"ps{2 + hp}"])
                        A([f"ps{0 + hp}"], [HB], out=hb3[:, 0, tau::SQ], in_=o_r, func=AF.Copy)
                        A([f"ps{2 + hp}"], [HB], out=hb3[:, 1, tau::SQ], in_=o_i, func=AF.Copy)
                    for ti, (n0, n1) in enumerate(NTILES):
                        b0, b1 = cmap(n0, n1)
                        pi = ti % 4
                        MM([dict(out=ps[pi][:, 0:n1 - n0], lhsT=v3(Cz[ri], 8)[:, tt, :], rhs=hb3[:, ri, b0:b1],
                                 start=(ri == 0), stop=(ri == 1)) for ri in range(2)], ["Cz0", "Cz1", HB], [f"ps{pi}"])
                        V("tensor_tensor", [f"ps{pi}", "yacc"], ["yacc"], out=yacc3[:, hfc, n0:n1], in0=yacc3[:, hfc, n0:n1],
                          in1=ps[pi][:, 0:n1 - n0], op=ALU.add)
                return stage2

            pend2 = None
            build_diag(0)
            for tt in range(8):
                dgp = itd % 2
                itd += 1
                nxt2 = s5_iter(tt, dgp)
                if pend2 is not None:
                    pend2()
                pend2 = nxt2
            pend2()
        wg = B.alloc(regB, 2 * 512, BF16)
        t.dma("pool", wg, wglu_d[l], w=["wglu"], sem="d_wglu")
        wg3 = v3(wg, 2)
        yg = B.alloc(regB, 2 * T, BF16)
        yg3 = v3(yg, 2)
        for c in range(2):
            V("scalar_tensor_tensor", [f"uT{c}", "yacc", "pvec"], ["yacc"], out=yacc3[:, c, :], in0=uT3[:, c, :],
              scalar=pv(l, "s5_d", c), in1=yacc3[:, c, :], op0=ALU.mult, op1=ALU.add)
            A(["yacc"], [f"yg{c}"], out=yg3[:, c, :], in_=yacc3[:, c, :], func=AF.Gelu_apprx_tanh)
        sg = [B.alloc(regB, 512) for _ in range(2)]
        for c in range(2):
            for ti, (n0, n1) in enumerate(NT_l):
                n = n1 - n0
                pa, pb = 3 + (ti % 2), 6 + (ti % 2)
                MM([dict(out=ps[pa][:, 0:n], lhsT=wg3[:, kt, c * 128:(c + 1) * 128], rhs=yg3[:, kt, n0:n1],
                         start=(kt == 0), stop=(kt == 1)) for kt in range(2)], ["wglu", "yg0", "yg1"], [f"ps{pa}"])
                MM([dict(out=ps[pb][:, 0:n], lhsT=wg3[:, kt, 256 + c * 128:256 + (c + 1) * 128], rhs=yg3[:, kt, n0:n1],
                         start=(kt == 0), stop=(kt == 1)) for kt in range(2)], ["wglu", "yg0", "yg1"], [f"ps{pb}"])
                A([f"ps{pb}", "pvec"], [f"sg{ti % 2}"], out=sg[ti % 2][:, 0:n], in_=ps[pb][:, 0:n], func=AF.Sigmoid,
                  bias=pv(l, "glu_b", 2 + c), scale=1.0)
                V("scalar_tensor_tensor", [f"ps{pa}", f"sg{ti % 2}", "pvec"], [f"mixbs{2 + c}"], out=mix3[:, 2 + c, n0:n1],
                  in0=ps[pa][:, 0:n], scalar=pv(l, "glu_b", c), in1=sg[ti % 2][:, 0:n], op0=ALU.add, op1=ALU.mult)
        if "s5" in B.dbg and l == 0:
            t.barrier()
            reset(regA)
            B.regD = regA
            B.dump_any("s5", mixbs[:, 2 * T:4 * T], 2 * T, ["mixbs2", "mixbs3"], BF16)
        t.barrier()
        if stop == "s5":
            break


        reset(regA)
        reset(regB)
        wslots_in = [B.alloc(regB, 8 * 128, BF16) for _ in range(3)]
        USW = 2364
        ustg = B.alloc(regA, 2 * USW, BF16)
        ustg3 = v3(ustg, 2)
        V("memset", [], ["ustg"], ap=ustg, constant=0.0)
        diagC = B.alloc(regA, 62 * 128, BF16)
        for c in range(2):
            for k in range(31):
                V("tensor_scalar", ["consts", "pvec"], ["diagC"], out=diagC[:, (c * 31 + k) * 128:(c * 31 + k + 1) * 128],
                  in0=ident_f, scalar1=pv(l, "dw_w", k * 2 + c), scalar2=None, op0=ALU.mult)
        wpw = B.alloc(regA, 512, BF16)
        t.dma("pool", wpw, wpw_d[l], w=["wpw"], sem="d_wpw")
        wpw3 = v3(wpw, 2)
        cv = B.alloc(regA, 2 * T)
        cv3 = v3(cv, 2)
        csb = B.alloc(regA, 2 * T, BF16)
        csb3 = v3(csb, 2)
        sig = [B.alloc(regB, 512) for _ in range(2)]
        sqf = [B.alloc(regB, 512) for _ in range(2)]
        mt_, m2_, var_, xn_ = [B.alloc(regB, 512) for _ in range(4)]
        it = 0
        for c in range(2):
            wtv, wkv = load_win_tile(l, J_CONF + c, wslots_in)
            wtg, wkg = load_win_tile(l, J_CONF + 2 + c, wslots_in)
            for ti, (n0, n1) in enumerate(NT_l):
                n = n1 - n0
                pa, pb = 3 + (it % 2), 5 + (it % 2)
                sgi = it % 2
                it += 1
                proj(wtv, wkv, n0, n1, ps[pa], f"ps{pa}")
                proj(wtg, wkg, n0, n1, ps[pb], f"ps{pb}")
                A([f"ps{pb}"], [f"sig{sgi}"], out=sig[sgi][:, 0:n], in_=ps[pb][:, 0:n], func=AF.Sigmoid)
                po = 15 + n0 if n0 < TC else n0 + 45
                V("tensor_tensor", [f"ps{pa}", f"sig{sgi}"], ["ustg"], out=ustg3[:, c, po:po + n], in0=ps[pa][:, 0:n],
                  in1=sig[sgi][:, 0:n], op=ALU.mult)
        it = 0
        for c in range(2):
            for ti, (n0, n1) in enumerate(NT_l):
                n = n1 - n0
                o0 = n0 if n0 < TC else n0 + 30
                pa = 3 + (it % 2)
                it += 1
                MM([dict(out=ps[pa][:, 0:n], lhsT=diagC[:, (c * 31 + k) * 128:(c * 31 + k + 1) * 128],
                         rhs=ustg3[:, c, o0 + k:o0 + k + n], start=(k == 0), stop=(k == 30)) for k in range(31)],
                   ["diagC", "ustg"], [f"ps{pa}"])
                A([f"ps{pa}", "pvec"], [f"cv{c}"], out=cv3[:, c, n0:n1], in_=ps[pa][:, 0:n], func=AF.Identity,
                  bias=pv(l, "dw_b", c), scale=1.0)
        for ti, (n0, n1) in enumerate(NT_l):
            n = n1 - n0
            for c in range(2):
                A([f"cv{c}"], [f"sqf{c}"], out=sqf[c][:, 0:n], in_=cv3[:, c, n0:n1], func=AF.Square)
            MM([dict(out=ps[1][:, 0:n], lhsT=ones_f, rhs=cv3[:, c, n0:n1], start=(c == 0), stop=(c == 1)) for c in range(2)],
               ["cv0", "cv1", "consts"], ["ps1"])
            MM([dict(out=ps[2][:, 0:n], lhsT=ones_f, rhs=sqf[c][:, 0:n], start=(c == 0), stop=(c == 1)) for c in range(2)],
               ["sqf0", "sqf1", "consts"], ["ps2"])
            V("tensor_scalar", ["ps1"], ["lnm"], out=mt_[:, 0:n], in0=ps[1][:, 0:n], scalar1=1.0 / 256.0, scalar2=None, op0=ALU.mult)
            V("tensor_tensor", ["lnm"], ["lnm2"], out=m2_[:, 0:n], in0=mt_[:, 0:n], in1=mt_[:, 0:n], op=ALU.mult)
            V("scalar_tensor_tensor", ["ps2", "lnm2"], ["lnv"], out=var_[:, 0:n], in0=ps[2][:, 0:n], scalar=1.0 / 256.0,
              in1=m2_[:, 0:n], op0=ALU.mult, op1=ALU.subtract)
            A(["lnv"], ["lnv"], out=var_[:, 0:n], in_=var_[:, 0:n], func=AF.Sqrt, bias=EPS, scale=1.0)
            V("reciprocal", ["lnv"], ["lnv"], out=var_[:, 0:n], in_=var_[:, 0:n])
            for c in range(2):
                V("tensor_tensor", [f"cv{c}", "lnm"], ["lnx"], out=xn_[:, 0:n], in0=cv3[:, c, n0:n1], in1=mt_[:, 0:n], op=ALU.subtract)
                V("tensor_tensor", ["lnx", "lnv"], ["lnx"], out=xn_[:, 0:n], in0=xn_[:, 0:n], in1=var_[:, 0:n], op=ALU.mult)
                A(["lnx", "pvec"], [f"csb{c}"], out=csb3[:, c, n0:n1], in_=xn_[:, 0:n], func=AF.Silu,
                  scale=pv(l, "ln_g", c), bias=pv(l, "ln_b", c))
            for c2 in range(2):
                pa = 3 + c2
                MM([dict(out=ps[pa][:, 0:n], lhsT=wpw3[:, c, c2 * 128:(c2 + 1) * 128], rhs=csb3[:, c, n0:n1],
                         start=(c == 0), stop=(c == 1)) for c in range(2)], ["wpw", "csb0", "csb1"], [f"ps{pa}"])
                A([f"ps{pa}", "pvec"], [f"mixbs{c2}"], out=mix3[:, c2, n0:n1], in_=ps[pa][:, 0:n], func=AF.Identity,
                  bias=pv(l, "pw_b", c2), scale=1.0)
        if "conf" in B.dbg and l == 0:
            t.barrier()
            reset(regA)
            B.regD = regA
            B.dump_any("conf", mixbs[:, 0:2 * T], 2 * T, ["mixbs0", "mixbs1"], BF16)
        t.barrier()
        if stop == "conf":
            break

        reset(regA)
        reset(regB)
        wslots_in = [B.alloc(regB, 8 * 128, BF16) for _ in range(3)]
        zS = B.alloc(regA, 4 * T, BF16)
        zS3 = v3(zS, 4)
        xbcT = B.alloc(regA, 8 * T, BF16)
        xbc3 = v3(xbcT, 8)
        PRW = 2312
        pre = [B.alloc(regA, PRW, BF16) for _ in range(2)]
        diagS = B.alloc(regB, 40 * 128, BF16)
        for j in range(8):
            for k in range(5):
                V("tensor_scalar", ["consts", "pvec"], ["diagS"], out=diagS[:, (j * 5 + k) * 128:(j * 5 + k + 1) * 128],
                  in0=ident_f, scalar1=pv(l, "conv_w", k * 8 + j), scalar2=None, op0=ALU.mult)
        for i in range(2):
            V("memset", [], [f"pre{i}"], ap=pre[i], constant=0.0)
        it = 0
        for j in range(4):
            wt, wk = load_win_tile(l, J_Z + j, wslots_in)
            for ti, (n0, n1) in enumerate(NT_l):
                pa = 3 + (it % 2)
                it += 1
                proj(wt, wk, n0, n1, ps[pa], f"ps{pa}")
                A([f"ps{pa}"], [f"zS{j}"], out=zS3[:, j, n0:n1], in_=ps[pa][:, 0:n1 - n0], func=AF.Silu)
        for j in range(8):
            wt, wk = load_win_tile(l, J_XBC + j, wslots_in)
            pr = pre[j % 2]
            pk = f"pre{j % 2}"
            for ti, (n0, n1) in enumerate(NTILES):
                pa = 3 + (it % 2)
                it += 1
                proj(wt, wk, n0, n1, ps[pa], f"ps{pa}")
                po = 2 + n0 if n0 < TC else n0 + 6
                A([f"ps{pa}"], [pk], out=pr[:, po:po + n1 - n0], in_=ps[pa][:, 0:n1 - n0], func=AF.Copy)
            for ti, (n0, n1) in enumerate(NTILES):
                n = n1 - n0
                o0 = n0 if n0 < TC else n0 + 4
                pa = 5 + (it % 2)
                it += 1
                MM([dict(out=ps[pa][:, 0:n], lhsT=diagS[:, (j * 5 + k) * 128:(j * 5 + k + 1) * 128], rhs=pr[:, o0 + k:o0 + k + n],
                         start=(k == 0), stop=(k == 4)) for k in range(5)], ["diagS", pk], [f"ps{pa}"])
                A([f"ps{pa}", "pvec"], [f"xbc{j}"], out=xbc3[:, j, n0:n1], in_=ps[pa][:, 0:n], func=AF.Silu,
                  bias=pv(l, "conv_b", j), scale=1.0)
        wt, wk = load_win_tile(l, J_DT, wslots_in)
        MM([dict(out=ps[1][:, c * 16:(c + 1) * 16], lhsT=hT3[:, kc, c * 128:(c + 1) * 128], rhs=wt[:, kc, 0:16],
                 start=(kc == 0), stop=(kc == 7)) for c in range(NCH) for kc in range(8)], [wk] + HK, ["ps1"])
        NX = NCH * 16
        xdt, e_, dtv, Atok, EcsmA, ErcsmA, Etot, dteF, dteB, tm_ = [B.alloc(regB, NX) for _ in range(10)]
        aneg = B.alloc(regB, 16)
        dtb = pbc[:, l * NPB:l * NPB + 16]
        alog = pbc[:, l * NPB + 16:l * NPB + 32]
        D_ = "dts"
        V("tensor_tensor", ["ps1", "pbc"], [D_], out=v3(xdt, NCH), in0=v3(ps[1][:, 0:NX], NCH),
          in1=dtb.unsqueeze(1).broadcast_to([128, NCH, 16]), op=ALU.add)
        V("tensor_scalar", [D_], [D_], out=e_, in0=xdt, scalar1=30.0, scalar2=None, op0=ALU.min)
        A([D_], [D_], out=e_, in_=e_, func=AF.Exp)
        A([D_], [D_], out=e_, in_=e_, func=AF.Ln, bias=1.0, scale=1.0)
        V("tensor_tensor", [D_], [D_], out=dtv, in0=e_, in1=xdt, op=ALU.max)
        A(["pbc", D_], [D_], out=aneg, in_=alog, func=AF.Exp)
        V("tensor_scalar", [D_], [D_], out=aneg, in0=aneg, scalar1=-1.0, scalar2=None, op0=ALU.mult)
        V("tensor_tensor", [D_], [D_], out=v3(Atok, NCH), in0=v3(dtv, NCH), in1=aneg.unsqueeze(1).broadcast_to([128, NCH, 16]), op=ALU.mult)
        MM([dict(out=ps[1][:, 0:NX], lhsT=LE_f, rhs=Atok)], [D_, "consts"], ["ps1"])
        MM([dict(out=ps[2][:, 0:NX], lhsT=GE_f, rhs=Atok)], [D_, "consts"], ["ps2"])
        MM([dict(out=ps[3][:, 0:NX], lhsT=ones_f, rhs=Atok)], [D_, "consts"], ["ps3"])
        A(["ps3"], [D_], out=Etot, in_=ps[3][:, 0:NX], func=AF.Exp)
        V("tensor_tensor", ["ps1", D_], [D_], out=tm_, in0=ps[1][:, 0:NX], in1=Atok, op=ALU.subtract)
        A([D_], [D_], out=EcsmA, in_=tm_, func=AF.Exp)
        V("tensor_tensor", ["ps2", D_], [D_], out=tm_, in0=ps[2][:, 0:NX], in1=Atok, op=ALU.subtract)
        A([D_], [D_], out=ErcsmA, in_=tm_, func=AF.Exp)
        V("tensor_tensor", [D_], [D_], out=v3(dteF, NCH)[:, :, 0:8], in0=v3(dtv, NCH)[:, :, 0:8], in1=v3(ErcsmA, NCH)[:, :, 0:8], op=ALU.mult)
        V("tensor_tensor", [D_], [D_], out=v3(dteB, NCH)[:, :, 0:8], in0=v3(dtv, NCH)[:, :, 8:16], in1=v3(EcsmA, NCH)[:, :, 8:16], op=ALU.mult)
        mark = regB[1]
        tok2 = [B.alloc(regB, 768, BF16) for _ in range(2)]
        Xb = [B.alloc(regB, 512, BF16) for _ in range(2)]
        Xdb = [B.alloc(regB, 512, BF16) for _ in range(2)]
        Wm = B.alloc(regB, 1024)
        expD = [B.alloc(regB, 1024, BF16) for _ in range(2)]
        einb = [B.alloc(regB, 1024, BF16) for _ in range(2)]
        Gm = [B.alloc(regB, 256, BF16) for _ in range(2)]
        MTb = [B.alloc(regB, 1024, BF16) for _ in range(2)]
        Ceb = [B.alloc(regB, 1024, BF16) for _ in range(2)]
        ytmp = B.alloc(regB, 512)
        S32 = B.alloc(regB, 512)
        Sb = B.alloc(regB, 512, BF16)
        it = 0
        S32s = [S32, B.alloc(regB, 512)]
        Sbs = [Sb, B.alloc(regB, 512, BF16)]
        for d in range(2):
            V("memset", [], [f"S32{d}"], ap=S32s[d], constant=0.0)
            V("memset", [], [f"Sb{d}"], ap=Sbs[d], constant=0.0)
        for j in range(4):
            V("tensor_scalar", [f"xbc{j}", "pvec"] + HK, [f"hT{j}"], out=hT3[:, j, :], in0=xbc3[:, j, :], scalar1=pv(l, "ssd_d", j),
              scalar2=None, op0=ALU.mult)
        orders = [list(range(NCH)), [1, 0] + list(range(NCH - 1, 1, -1))]
        def ssd_iter(step, d, p_):
            hoff = 8 * d
            maskW = LE_f if d == 0 else GE_f
            Dl = GT_f if d == 0 else LT_f
            dte = dteF if d == 0 else dteB
            S32, Sb = S32s[d], Sbs[d]
            SK32, SKb = f"S32{d}", f"Sb{d}"
            c = orders[d][step]
            c0, c1 = c * 128, (c + 1) * 128
            V("tensor_tensor", ["consts", D_], ["Wm"], out=v3(Wm, 8), in0=maskW.unsqueeze(1).broadcast_to([128, 8, 128]),
              in1=v3(Atok, NCH)[:, c, hoff:hoff + 8].unsqueeze(2).broadcast_to([128, 8, 128]), op=ALU.mult)
            psb = ps[3][:, 0:384].bitcast(BF16)
            TR([dict(out=psb[:, i * 128:(i + 1) * 128], in_=xbc3[:, i, c0:c1], ident=ident_b) for i in range(6)],
               [f"xbc{i}" for i in range(6)] + ["cb"], ["ps3"])
            A(["ps3"], [f"tok{p_}"], out=tok2[p_], in_=psb, func=AF.Copy)
            xs_tok, B_tok = tok2[p_][:, 0:512], tok2[p_][:, 512:768]
            V("tensor_tensor", [f"tok{p_}", D_], [f"X{p_}"], out=v3(Xb[p_], 8), in0=v3(xs_tok, 8),
              in1=v3(dtv, NCH)[:, c, hoff:hoff + 8].unsqueeze(2).broadcast_to([128, 8, 64]), op=ALU.mult)
            V("tensor_tensor", [f"tok{p_}", D_], [f"Xd{p_}"], out=v3(Xdb[p_], 8), in0=v3(xs_tok, 8),
              in1=v3(dte, NCH)[:, c, 0:8].unsqueeze(2).broadcast_to([128, 8, 64]), op=ALU.mult)
            MM([dict(out=ps[4][:, 0:512], lhsT=Dl, rhs=Wm[:, 0:512]), dict(out=ps[5][:, 0:512], lhsT=Dl, rhs=Wm[:, 512:1024])],
               ["Wm", "consts"], ["ps4", "ps5"])
            MM([dict(out=ps[6][:, 0:512], lhsT=ones_f, rhs=Wm[:, 0:512]), dict(out=ps[7][:, 0:512], lhsT=ones_f, rhs=Wm[:, 512:1024])],
               ["Wm", "consts"], ["ps6", "ps7"])
            A(["ps4"], [f"expD{p_}a"], out=expD[p_][:, 0:512], in_=ps[4][:, 0:512], func=AF.Exp)
            A(["ps5"], [f"expD{p_}b"], out=expD[p_][:, 512:1024], in_=ps[5][:, 0:512], func=AF.Exp)
            A(["ps6"], [f"ein{p_}a"], out=einb[p_][:, 0:512], in_=ps[6][:, 0:512], func=AF.Exp)
            A(["ps7"], [f"ein{p_}b"], out=einb[p_][:, 512:1024], in_=ps[7][:, 0:512], func=AF.Exp)
            MM([dict(out=ps[0][:, g * 128:(g + 1) * 128], lhsT=xbc3[:, 4 + g, c0:c1], rhs=xbc3[:, 6 + g, c0:c1]) for g in range(2)],
               ["xbc4", "xbc5", "xbc6", "xbc7"], ["ps0"])
            V("tensor_tensor", ["ps0", "consts"], [f"Gm{p_}"], out=v3(Gm[p_], 2), in0=v3(ps[0][:, 0:256], 2),
              in1=maskW.unsqueeze(1).broadcast_to([128, 2, 128]), op=ALU.mult)
            V("tensor_tensor", [f"expD{p_}a", f"expD{p_}b", f"Gm{p_}"], [f"MT{p_}"], out=v4(MTb[p_], 2, 4), in0=v4(expD[p_], 2, 4),
              in1=v3(Gm[p_], 2).unsqueeze(2).broadcast_to([128, 2, 4, 128]), op=ALU.mult)
            V("tensor_tensor", [f"ein{p_}a", f"ein{p_}b", "xbc6", "xbc7"], [f"Ce{p_}"], out=v4(Ceb[p_], 2, 4), in0=v4(einb[p_], 2, 4),
              in1=xbc3[:, 6:8, c0:c1].unsqueeze(2).broadcast_to([128, 2, 4, 128]), op=ALU.mult)

            def stageB():
                mms = []
                for h in range(8):
                    o_ = ps[1][(h % 2) * 64:(h % 2) * 64 + 64, (h // 2) * 128:(h // 2 + 1) * 128]
                    mms.append(dict(out=o_, lhsT=Xb[p_][:, h * 64:(h + 1) * 64], rhs=MTb[p_][:, h * 128:(h + 1) * 128], start=True, stop=False))
                    mms.append(dict(out=o_, lhsT=Sb[:, h * 64:(h + 1) * 64], rhs=Ceb[p_][:, h * 128:(h + 1) * 128], start=False, stop=True))
                MM(mms, [f"X{p_}", f"MT{p_}", SKb, f"Ce{p_}"], ["ps1"])
                yk = [f"hT{j}" for j in range(4)]
                V("tensor_tensor", yk + ["ps1"], yk, out=hT3[:, 0:4, c0:c1], in0=hT3[:, 0:4, c0:c1], in1=v3(ps[1][:, 0:512], 4), op=ALU.add)
                MM([dict(out=ps[2][:, g * 256:(g + 1) * 256], lhsT=B_tok[:, g * 128:(g + 1) * 128], rhs=Xdb[p_][:, g * 256:(g + 1) * 256])
                    for g in range(2)], [f"tok{p_}", f"Xd{p_}"], ["ps2"])
                V("tensor_tensor", [SK32, D_], [SK32], out=v3(S32, 8), in0=v3(S32, 8),
                  in1=v3(Etot, NCH)[:, c, hoff:hoff + 8].unsqueeze(2).broadcast_to([128, 8, 64]), op=ALU.mult)
                V("tensor_tensor", [SK32, "ps2"], [SK32], out=S32, in0=S32, in1=ps[2][:, 0:512], op=ALU.add)
                V("tensor_copy", [SK32], [SKb], out=Sb, in_=S32)
            return stageB

        pendB = None
        for step in range(NCH):
            for d in range(2):
                nxtB = ssd_iter(step, d, it % 2)
                it += 1
                if pendB is not None:
                    pendB()
                pendB = nxtB
        pendB()
        t.barrier()
        regB[1] = mark
        y2 = B.alloc(regB, 4 * 512)
        sq4 = B.alloc(regB, 4 * 512, BF16)
        rs_ = B.alloc(regB, 512)
        for ti, (n0, n1) in enumerate(NT_l):
            n = n1 - n0
            V("tensor_tensor", [f"hT{j}" for j in range(4)] + [f"zS{j}" for j in range(4)], ["y2"], out=v3(y2, 4)[:, :, 0:n],
              in0=hT3[:, 0:4, n0:n1], in1=zS3[:, :, n0:n1], op=ALU.mult)
            A(["y2"], ["sq4"], out=v3(sq4, 4)[:, :, 0:n], in_=v3(y2, 4)[:, :, 0:n], func=AF.Square)
            MM([dict(out=ps[1][:, 0:n], lhsT=ones_b, rhs=v3(sq4, 4)[:, j, 0:n], start=(j == 0), stop=(j == 3)) for j in range(4)],
               ["sq4", "cb"], ["ps1"])
            A(["ps1"], ["rs_"], out=rs_[:, 0:n], in_=ps[1][:, 0:n], func=AF.Sqrt, bias=EPS, scale=1.0 / 512.0)
            V("reciprocal", ["rs_"], ["rs_"], out=rs_[:, 0:n], in_=rs_[:, 0:n])
            for j in range(4):
                V("scalar_tensor_tensor", ["y2", "rs_", "pvec"], [f"hT{j}"], out=hT3[:, j, n0:n1], in0=v3(y2, 4)[:, j, 0:n],
                  scalar=pv(l, "ssd_ng", j), in1=rs_[:, 0:n], op0=ALU.mult, op1=ALU.mult)
        if "ssd" in B.dbg and l == 0:
            t.barrier()
            reset(regA)
            B.regD = regA
            B.dump_any("ssd", hT[:, 0:4 * T], 4 * T, [f"hT{j}" for j in range(4)], BF16)
        t.barrier()
        if stop == "ssd":
            break

        reset(regA)
        reset(regB)
        for k in range(8):
            t.dma("sp", x3[:, k, :], xsp[:, k * T:(k + 1) * T], r=[f"xsp{k}"], w=[f"xT{k}"])
        wo = B.alloc(regB, 8 * 1024, BF16)
        wo3 = v3(wo, 8)
        for i in range(2):
            t.dma("pool", wo[:, i * 4096:(i + 1) * 4096], wout_d[l][:, i * 4096:(i + 1) * 4096], w=[f"wo{i}"])
        it = 0
        for ti, (n0, n1) in enumerate(NT_l):
            n = n1 - n0
            col = 1 if n0 < TC else 0
            for dch in range(8):
                pa = 3 + (it % 4)
                it += 1
                mms = []
                for kt in range(8):
                    rhs = hT3[:, kt, n0:n1] if kt < 4 else mix3[:, kt - 4, n0:n1]
                    mms.append(dict(out=ps[pa][:, 0:n], lhsT=wo3[:, kt, dch * 128:(dch + 1) * 128], rhs=rhs, start=(kt == 0), stop=(kt == 7)))
                MM(mms, ["wo0", "wo1"] + [f"hT{j}" for j in range(4)] + [f"mixbs{j}" for j in range(4)], [f"ps{pa}"])
                V("scalar_tensor_tensor", [f"ps{pa}", "modT", f"xT{dch}"], [f"xT{dch}"], out=x3[:, dch, n0:n1], in0=ps[pa][:, 0:n],
                  scalar=modc(l, 16 + dch, col), in1=x3[:, dch, n0:n1], op0=ALU.mult, op1=ALU.add)
        if "xmid" in B.dbg and l == 0:
            B.dump("xmid", xT, 8 * T, XK)
        t.barrier()
        if stop == "xmid":
            break

        reset(regMB)
        tokssq = B.alloc(regMB, NCH)
        GTb = B.alloc(regMB, T, BF16)
        selE = B.alloc(regMB, 16 * 128, BF16)
        mark = regMB[1]
        rmsnorm_to_h(l, 1, regMB, tok_ssq=tokssq, ntiles=NT_l)
        NG = NCH * 16
        rwm = [B.alloc(regMB, 128) for _ in range(2)]
        shb = [B.alloc(regMB, 8 * 128) for _ in range(2)]
        lg, pm, tq, e1, e2 = [B.alloc(regMB, NG) for _ in range(5)]
        cbias = B.alloc(regMB, 32)
        rst = B.alloc(regMB, NCH)
        sm2 = B.alloc(regMB, 24 * NCH * 4)
        R_ = "rt"
        rb = pbc[:, l * NPB + 32:l * NPB + 48]
        der2 = v3(der[:, l * 32 + 16:l * 32 + 32], 8)
        for col in range(2):
            V("tensor_tensor", ["rwf", "der"], [R_], out=v3(rwm[col], 8), in0=v3(rwf, 8), in1=der2[:, :, col:col + 1].broadcast_to([128, 8, 16]), op=ALU.mult)
            for k in range(8):
                V("tensor_copy", ["modT"], [R_], out=shb[col][:, k * 128:(k + 1) * 128], in_=modc(l, 24 + k, col).broadcast_to([128, 128]))
            MM([dict(out=ps[3][:, col * 16:(col + 1) * 16], lhsT=shb[col][:, k * 128:(k + 1) * 128], rhs=v3(rwf, 8)[:, k, :],
                     start=(k == 0), stop=(k == 7)) for k in range(8)], [R_, "rwf"], ["ps3"])
        V("tensor_tensor", ["ps3", "pbc"], [R_], out=v3(cbias, 2), in0=v3(ps[3][:, 0:32], 2), in1=rb.unsqueeze(1).broadcast_to([128, 2, 16]), op=ALU.add)
        MM([dict(out=ps[4][:, c * 16:(c + 1) * 16], lhsT=x3[:, k, c * 128:(c + 1) * 128], rhs=v3(rwm[1 if c < 2 else 0], 8)[:, k, :],
                 start=(k == 0), stop=(k == 7)) for c in range(NCH) for k in range(8)], [R_] + XK, ["ps4"])
        A(["tokssq"], [R_], out=rst, in_=tokssq, func=AF.Sqrt, bias=1024.0 * EPS, scale=1.0)
        V("reciprocal", [R_], [R_], out=rst, in_=rst)
        lg3, pm3, tq3, e13, e23 = [v3(a_, NCH) for a_ in (lg, pm, tq, e1, e2)]
        V("tensor_tensor", ["ps4", R_], [R_], out=lg3, in0=v3(ps[4][:, 0:NG], NCH), in1=rst.unsqueeze(2).broadcast_to([128, NCH, 16]), op=ALU.mult)
        V("tensor_tensor", [R_], [R_], out=lg3[:, 0:2, :], in0=lg3[:, 0:2, :], in1=cbias[:, 16:32].unsqueeze(1).broadcast_to([128, 2, 16]), op=ALU.add)
        V("tensor_tensor", [R_], [R_], out=lg3[:, 2:NCH, :], in0=lg3[:, 2:NCH, :], in1=cbias[:, 0:16].unsqueeze(1).broadcast_to([128, NCH - 2, 16]), op=ALU.add)

        def sm(i, w_=1):
            return sm2[:, i * NCH * 4:i * NCH * 4 + NCH * w_]

        def rmax(dst, src3, width):
            V("tensor_tensor", [R_], [R_], out=dst, in0=src3[:, :, 0], in1=src3[:, :, 1], op=ALU.max)
            for i in range(2, width):
                V("tensor_tensor", [R_], [R_], out=dst, in0=dst, in1=src3[:, :, i], op=ALU.max)

        mx = sm(0)
        rmax(mx, lg3, 16)
        V("tensor_tensor", [R_], [R_], out=pm3, in0=lg3, in1=mx.unsqueeze(2).broadcast_to([128, NCH, 16]), op=ALU.subtract)
        A([R_], [R_], out=pm, in_=pm, func=AF.Exp)
        p4 = pm.rearrange("p (c g j) -> p c g j", c=NCH, g=4)
        gs = sm(1, 4)
        gs3 = v3(gs, NCH)
        pair = sm(2, 4)
        pair3 = v3(pair, NCH)
        first = True
        for a_ in range(4):
            for b_ in range(a_ + 1, 4):
                if first:
                    V("tensor_tensor", [R_], [R_], out=gs3, in0=p4[:, :, :, a_], in1=p4[:, :, :, b_], op=ALU.add)
                    first = False
                else:
                    V("tensor_tensor", [R_], [R_], out=pair3, in0=p4[:, :, :, a_], in1=p4[:, :, :, b_], op=ALU.add)
                    V("tensor_tensor", [R_], [R_], out=gs3, in0=gs3, in1=pair3, op=ALU.max)
        gmx = sm(3)
        rmax(gmx, gs3, 4)
        eq = sm(4, 4)
        eq3 = v3(eq, NCH)
        V("tensor_tensor", [R_], [R_], out=eq3, in0=gs3, in1=gmx.unsqueeze(2).broadcast_to([128, NCH, 4]), op=ALU.is_equal)
        rem = sm(5)
        selg = sm(6, 4)
        selg3 = v3(selg, NCH)
        V("tensor_copy", [R_], [R_], out=selg3[:, :, 0], in_=eq3[:, :, 0])
        V("tensor_scalar", [R_], [R_], out=rem, in0=eq3[:, :, 0], scalar1=-1.0, scalar2=1.0, op0=ALU.mult, op1=ALU.add)
        for g_ in range(1, 4):
            V("tensor_tensor", [R_], [R_], out=selg3[:, :, g_], in0=eq3[:, :, g_], in1=rem, op=ALU.mult)
            if g_ < 3:
                V("tensor_tensor", [R_], [R_], out=rem, in0=rem, in1=selg3[:, :, g_], op=ALU.subtract)
        V("tensor_tensor", [R_], [R_], out=p4, in0=p4, in1=selg3.unsqueeze(3).broadcast_to([128, NCH, 4, 4]), op=ALU.mult)
        v1 = sm(7)
        rmax(v1, pm3, 16)
        V("tensor_tensor", [R_], [R_], out=e13, in0=pm3, in1=v1.unsqueeze(2).broadcast_to([128, NCH, 16]), op=ALU.is_equal)
        V("tensor_tensor", [R_], [R_], out=tq3, in0=e13, in1=pm3, op=ALU.mult)
        V("tensor_tensor", [R_], [R_], out=tq3, in0=pm3, in1=tq3, op=ALU.subtract)
        v2 = sm(8)
        rmax(v2, tq3, 16)
        V("tensor_tensor", [R_], [R_], out=e23, in0=tq3, in1=v2.unsqueeze(2).broadcast_to([128, NCH, 16]), op=ALU.is_equal)
        V("tensor_tensor", [R_], [R_], out=e13, in0=e13, in1=e23, op=ALU.add)
        V("tensor_tensor", [R_], [R_], out=e13, in0=e13, in1=pm3, op=ALU.mult)
        V("tensor_tensor", [R_], [R_], out=v1, in0=v1, in1=v2, op=ALU.add)
        V("reciprocal", [R_], [R_], out=v1, in_=v1)
        V("tensor_tensor", [R_], [R_], out=e13, in0=e13, in1=v1.unsqueeze(2).broadcast_to([128, NCH, 16]), op=ALU.mult)
        for grp in range(5):
            cs_ = list(range(grp * 4, min(NCH, grp * 4 + 4)))
            TR([dict(out=ps[5][0:16, i * 128:(i + 1) * 128], in_=e13[:, c, :], ident=ident_f) for i, c in enumerate(cs_)],
               [R_, "consts"], ["ps5"])
            n_ = len(cs_) * 128
            A(["ps5"], ["GTb"], out=GTb[0:16, grp * 512:grp * 512 + n_], in_=ps[5][0:16, 0:n_], func=AF.Copy)
        V("tensor_copy", ["consts"], ["selE"], out=v3(selE, 16)[0:16], in_=ident_f[0:16, 0:16].unsqueeze(2).broadcast_to([16, 16, 128]))
        if "gates" in B.dbg and l == 0:
            B.dump("gates", e1, NG, [R_])
        t.barrier()
        if stop == "router":
            break

        regMB[1] = mark
        wexp = B.wexp_ap()
        wsl = [B.alloc(regMB, 3 * 4096, BF16) for _ in range(2)]
        GBb = [B.alloc(regMB, T, BF16) for _ in range(2)]
        actb = [B.alloc(regMB, 4 * 512, BF16) for _ in range(2)]
        silb = [B.alloc(regMB, 512) for _ in range(2)]
        ui = 0
        ci = 0
        pend = None
        item = 0

        def emit_down(pd_):
            Wd3_, act3_, apk, wk_, n0_, n1_, col_ = pd_
            n_ = n1_ - n0_
            for dch in range(8):
                pdb = 4 + (dch % 2)
                MM([dict(out=ps[pdb][:, 0:n_], lhsT=Wd3_[:, m, dch * 128:(dch + 1) * 128], rhs=act3_[:, m, 0:n_], start=(m == 0), stop=(m == 3))
                    for m in range(4)], [wk_, apk], [f"ps{pdb}"])
                V("scalar_tensor_tensor", [f"ps{pdb}", "modT", f"xT{dch}"], [f"xT{dch}"], out=x3[:, dch, n0_:n1_], in0=ps[pdb][:, 0:n_],
                  scalar=modc(l, 40 + dch, col_), in1=x3[:, dch, n0_:n1_], op0=ALU.mult, op1=ALU.add)

        for e in range(16):
            gp = e % 2
            for ti, (n0, n1) in enumerate(NT_l):
                n = n1 - n0
                MM([dict(out=ps[7][:, 0:n], lhsT=selE[0:16, e * 128:(e + 1) * 128], rhs=GTb[0:16, n0:n1])], ["selE", "GTb"], ["ps7"])
                A(["ps7"], [f"GB{gp}"], out=GBb[gp][:, n0:n1], in_=ps[7][:, 0:n], func=AF.Copy)
            for q in range(2):
                s = ui % 2
                ui += 1
                for i in range(3):
                    t.dma("pool", wsl[s][:, i * 4096:(i + 1) * 4096], wexp[l * 32 + e * 2 + q][:, i * 4096:(i + 1) * 4096],
                          w=[f"wx{s}_{i}"])
                Wg3, Wu3, Wd3 = v3(wsl[s][:, 0:4096], 8), v3(wsl[s][:, 4096:8192], 8), v3(wsl[s][:, 8192:12288], 4)
                for ti, (n0, n1) in enumerate(NT_l):
                    n = n1 - n0
                    col = 1 if n0 < TC else 0
                    pa_ = item % 2
                    item += 1
                    act3 = v3(actb[pa_], 4)
                    for m in range(4):
                        pg, pu = ci % 2, 2 + (ci % 2)
                        sl = silb[ci % 2]
                        sk = f"sil{ci % 2}"
                        ci += 1
                        MM([dict(out=ps[pg][:, 0:n], lhsT=Wg3[:, kc, m * 128:(m + 1) * 128], rhs=hT3[:, kc, n0:n1], start=(kc == 0), stop=(kc == 7))
                            for kc in range(8)], [f"wx{s}_0"] + HK, [f"ps{pg}"])
                        MM([dict(out=ps[pu][:, 0:n], lhsT=Wu3[:, kc, m * 128:(m + 1) * 128], rhs=hT3[:, kc, n0:n1], start=(kc == 0), stop=(kc == 7))
                            for kc in range(8)], [f"wx{s}_1"] + HK, [f"ps{pu}"])
                        A([f"ps{pg}"], [sk], out=sl[:, 0:n], in_=ps[pg][:, 0:n], func=AF.Silu)
                        V("tensor_tensor", [sk, f"ps{pu}"], [sk], out=sl[:, 0:n], in0=sl[:, 0:n], in1=ps[pu][:, 0:n], op=ALU.mult)
                        V("tensor_tensor", [sk, f"GB{gp}"], [f"act{pa_}"], out=act3[:, m, 0:n], in0=sl[:, 0:n], in1=GBb[gp][:, n0:n1], op=ALU.mult)
                    if pend is not None:
                        emit_down(pend)
                    pend = (Wd3, act3, f"act{pa_}", f"wx{s}_2", n0, n1, col)
        emit_down(pend)
        if "xend" in B.dbg and l == 0:
            B.dump("xend", xT, 8 * T, XK)
        t.barrier()

    if stop is None:
        reset(regMB)
        sqb = B.alloc(regMB, 8 * 512, BF16)
        sqb3 = v3(sqb, 8)
        rsf = [B.alloc(regMB, 512) for _ in range(2)]
        ob = [B.alloc(regMB, 512) for _ in range(4)]
        gf = B.alloc(regMB, 8)
        V("tensor_scalar", ["pvec"], ["gf"], out=gf, in0=pv(0, "g_final", 0, 8), scalar1=32.0, scalar2=None, op0=ALU.mult)
        oi = 0
        for ti, (n0, n1) in enumerate(NTILES[1:]):
            n = n1 - n0
            for k in range(8):
                if k % 2 == 0:
                    A([f"xT{k}"], [f"sqb{k}"], out=sqb3[:, k, 0:n], in_=x3[:, k, n0:n1], func=AF.Square)
                else:
                    V("tensor_tensor", [f"xT{k}"], [f"sqb{k}"], out=sqb3[:, k, 0:n], in0=x3[:, k, n0:n1], in1=x3[:, k, n0:n1], op=ALU.mult)
            MM([dict(out=ps[1][:, 0:n], lhsT=ones_b, rhs=sqb3[:, k, 0:n], start=(k == 0), stop=(k == 7)) for k in range(8)],
               [f"sqb{k}" for k in range(8)] + ["cb"], ["ps1"])
            r_ = rsf[ti % 2]
            rk = f"rsf{ti % 2}"
            A(["ps1"], [rk], out=r_[:, 0:n], in_=ps[1][:, 0:n], func=AF.Sqrt, bias=1024.0 * EPS, scale=1.0)
            V("reciprocal", [rk], [rk], out=r_[:, 0:n], in_=r_[:, 0:n])
            for k in range(8):
                o_ = ob[oi % 4]
                ok = f"ob{oi % 4}"
                oi += 1
                V("scalar_tensor_tensor", [f"xT{k}", rk, "gf"], [ok], out=o_[:, 0:n], in0=x3[:, k, n0:n1], scalar=gf[:, k:k + 1],
                  in1=r_[:, 0:n], op0=ALU.mult, op1=ALU.mult)
                t.dma("sp", out_d[:, k * 2048 + n0 - TC:k * 2048 + n1 - TC], o_[:, 0:n], r=[ok], w=[], sem=f"d_ob{oi % 4}")
    t.barrier()
    return B


def _fm(a):
    n = a.shape[0]
    return np.ascontiguousarray(a.T.reshape(8, 128, n).transpose(1, 0, 2).reshape(128, 8 * n))


def _cols(v, nt):
    return np.asarray(v, np.float32).reshape(nt, 128).T


def prep_shared(inp):
    f = lambda k: np.asarray(inp[k], np.float32)
    sh = {}
    w_ada = f("w_ada")
    sh["wada"] = np.ascontiguousarray(
        w_ada.reshape(NL, 8, 128, 12, 512).transpose(0, 3, 2, 1, 4).reshape(NL * 12, 128, 8 * 512))
    pvec = np.zeros((128, NL, NPV), np.float32)
    pbc = np.zeros((128, NL, NPB), np.float32)
    for l in range(NL):
        def put(name, arr):
            arr = np.asarray(arr, np.float32)
            pvec[:, l, PV[name]:PV[name] + arr.shape[1]] = arr
        put("b_ada", _cols(f("b_ada")[l], 48))
        put("g_mix", _cols(f("g_mix")[l], 8))
        put("g_ffn", _cols(f("g_ffn")[l], 8))
        cw = f("ssd_conv_w")[l]
        put("conv_w", np.concatenate([_cols(cw[k], 8) for k in range(5)], axis=1))
        put("conv_b", _cols(f("ssd_conv_b")[l], 8))
        put("ssd_d", _cols(np.repeat(f("ssd_d")[l], 64), 4))
        put("ssd_ng", _cols(f("ssd_norm_g")[l], 4))
        dw = f("conf_dw_w")[l]
        put("dw_w", np.concatenate([_cols(dw[k], 2) for k in range(31)], axis=1))
        put("dw_b", _cols(f("conf_dw_b")[l], 2))
        put("ln_g", _cols(f("conf_ln_g")[l], 2))
        put("ln_b", _cols(f("conf_ln_b")[l], 2))
        put("pw_b", _cols(f("conf_pw_b")[l], 2))
        put("s5_d", _cols(f("s5_d")[l], 2))
        put("glu_b", _cols(f("s5_glu_b")[l], 4))
        put("g_final", _cols(f("g_final"), 8))
        put("lamre", np.concatenate([_cols(f("s5_lambda_re")[l, d].reshape(-1), 8) for d in range(2)], axis=1))
        put("lamim", np.concatenate([_cols(f("s5_lambda_im")[l, d].reshape(-1), 8) for d in range(2)], axis=1))
        put("lstep", np.concatenate([_cols(np.repeat(f("s5_log_step")[l, d], 64), 8) for d in range(2)], axis=1))
        put("bre", f("s5_b_re")[l].reshape(8, 128, 16).transpose(1, 0, 2).reshape(128, 128))
        put("bim", f("s5_b_im")[l].reshape(8, 128, 16).transpose(1, 0, 2).reshape(128, 128))
        put("cre", f("s5_c_re")[l].transpose(0, 2, 1).reshape(8, 128, 16).transpose(1, 0, 2).reshape(128, 128))
        put("cim", f("s5_c_im")[l].transpose(0, 2, 1).reshape(8, 128, 16).transpose(1, 0, 2).reshape(128, 128))
        pbc[:, l, 0:16] = f("ssd_dt_bias")[l].reshape(1, 16)
        pbc[:, l, 16:32] = f("ssd_a_log")[l].reshape(1, 16)
        pbc[:, l, 32:48] = f("router_b").reshape(1, 16)
    sh["pvec"] = pvec.reshape(128, NL * NPV)
    sh["pbc"] = pbc.reshape(128, NL * NPB)
    k = np.arange(128)
    le = (k[:, None] <= k[None, :]).astype(np.float32)
    sh["consts"] = np.concatenate([np.eye(128, dtype=np.float32), np.ones((128, 128), np.float32), le, 1 - le,
                                   (k[:, None] >= k[None, :]).astype(np.float32),
                                   (k[:, None] < k[None, :]).astype(np.float32)], axis=1)
    w_in = f("w_in")
    win = np.zeros((NL, 19, 128, 8, 128), np.float32)
    for j, (c0, n) in enumerate(WIN_TILES):
        win[:, j, :, :, :n] = w_in[:, :, c0:c0 + n].reshape(NL, 8, 128, n).transpose(0, 2, 1, 3)
    sh["win"] = win.reshape(NL * 19, 128, 8 * 128)
    sh["rw"] = np.ascontiguousarray(f("router_w").reshape(8, 128, 16).transpose(1, 0, 2).reshape(128, 128))
    sh["wpw"] = np.ascontiguousarray(f("conf_pw_w").reshape(NL, 2, 128, 256).transpose(0, 2, 1, 3).reshape(NL, 128, 512))
    sh["wglu"] = np.ascontiguousarray(f("s5_glu_w").reshape(NL, 2, 128, 512).transpose(0, 2, 1, 3).reshape(NL, 128, 1024))
    sh["wout"] = np.ascontiguousarray(f("w_out").reshape(NL, 8, 128, 1024).transpose(0, 2, 1, 3).reshape(NL, 128, 8192))
    return sh


def prep_experts(inp):
    wexp = np.empty((NL, 16, 2, 128, 3, 4096), np.float32)
    for i, name in enumerate(("exp_w_gate", "exp_w_up")):
        w = np.asarray(inp[name], np.float32).reshape(NL, 16, 8, 128, 2, 512)
        wexp[:, :, :, :, i, :] = w.transpose(0, 1, 4, 3, 2, 5).reshape(NL, 16, 2, 128, 4096)
    w = np.asarray(inp["exp_w_down"], np.float32).reshape(NL, 16, 2, 4, 128, 1024)
    wexp[:, :, :, :, 2, :] = w.transpose(0, 1, 2, 4, 3, 5).reshape(NL, 16, 2, 128, 4096)
    return wexp.reshape(NL * 32, 128, 3 * 4096)


def prep_core(inp, b):
    xc = np.concatenate([np.asarray(inp["ctx"][b], np.float32), np.asarray(inp["x"][b], np.float32)], axis=0)
    cond = np.stack([_cols(inp["c"][b], 8), _cols(inp["c_ctx"], 8)], axis=2).reshape(128, 16)
    return {"xin": _fm(xc), "cond": np.ascontiguousarray(cond, dtype=np.float32)}


def kernel(**inputs):
    nc = bass.Bass("TRN2", target_bir_lowering=False)
    with contextlib.ExitStack() as es:
        build(nc, es)
    sh = prep_shared(inputs)
    sh["wexp"] = prep_experts(inputs)
    in_maps = []
    for b in range(8):
        m = dict(sh)
        m.update(prep_core(inputs, b))
        in_maps.append(m)
    res = run_bass_kernel_spmd(nc, in_maps, core_ids=list(range(8)))
    outs = [r["out"].reshape(128, 8, 2048).transpose(2, 1, 0).reshape(2048, 1024) for r in res.results]
    return np.stack(outs, axis=0).astype(np.float32)
```
